# Optimizing a Trainium2 kernel written in Bass

```python
import jax, jax.numpy as jnp
from jax import lax
import numpy as np

D_MODEL = 1024
BATCH = 8
SEQ = 4096
DEPTH = 2

FOX_HEADS = 8
FOX_HEAD_DIM = 64
FOX_WIDTH = FOX_HEADS * FOX_HEAD_DIM
MLA_HEADS = 8
MLA_NOPE_DIM = 64
MLA_ROPE_DIM = 32
MLA_V_DIM = 64
MLA_WIDTH = MLA_HEADS * MLA_V_DIM
D_MIX = FOX_WIDTH + MLA_WIDTH
Q_LORA_RANK = 256
KV_LORA_RANK = 128
ROPE_THETA = 10000.0
IN_WIDTH = 3 * FOX_WIDTH + FOX_HEADS + Q_LORA_RANK + KV_LORA_RANK + MLA_ROPE_DIM
Q_BLOCK = 128
N_GROUPS = 8
EXPERTS_PER_GROUP = 4
N_EXPERTS = N_GROUPS * EXPERTS_PER_GROUP
TOP_K_IN_GROUP = 2
D_EXPERT = 256
EXPERT_BLOCK = 128
NORM_EPS = 1e-6

kernel_name = 'hybrid_fox_mla_hmoe'


def rms_norm(x, gain):
    xf = x.astype(jnp.float32)
    y = xf * lax.rsqrt(jnp.mean(xf * xf, axis=-1, keepdims=True) + NORM_EPS)
    return (y * gain.astype(jnp.float32)).astype(x.dtype)


def rotary(x, positions):
    half = MLA_ROPE_DIM // 2
    inv_freq = ROPE_THETA ** (-jnp.arange(half, dtype=jnp.float32) / half)
    ang = positions.astype(jnp.float32)[..., None] * inv_freq
    ang = ang.reshape(ang.shape[:2] + (1,) * (x.ndim - 3) + (half,))
    cos, sin = jnp.cos(ang), jnp.sin(ang)
    xf = x.astype(jnp.float32)
    x1, x2 = xf[..., :half], xf[..., half:]
    return jnp.concatenate([x1 * cos - x2 * sin, x2 * cos + x1 * sin], axis=-1).astype(x.dtype)


def causal_block_attention(q, k, v, log_forget_cum=None):
    B, S, H, dk = q.shape
    dv = v.shape[-1]
    nb = S // Q_BLOCK
    scale = dk ** -0.5
    kf = k.astype(jnp.float32)
    vf = v.astype(jnp.float32)
    key_pos = jnp.arange(S)
    q_blocks = q.reshape(B, nb, Q_BLOCK, H, dk).transpose(1, 0, 2, 3, 4)
    has_decay = log_forget_cum is not None
    if has_decay:
        d_keys = log_forget_cum.transpose(0, 2, 1)
        d_blocks = log_forget_cum.reshape(B, nb, Q_BLOCK, H).transpose(1, 0, 3, 2)
        xs = (jnp.arange(nb), q_blocks, d_blocks)
    else:
        xs = (jnp.arange(nb), q_blocks)

    def one_block(args):
        i, q_i = args[0], args[1]
        s = jnp.einsum('bqhd,bkhd->bhqk', q_i.astype(jnp.float32), kf) * scale
        if has_decay:
            s = s + args[2][..., :, None] - d_keys[:, :, None, :]
        q_pos = i * Q_BLOCK + jnp.arange(Q_BLOCK)
        s = jnp.where(key_pos[None, :] <= q_pos[:, None], s, -jnp.inf)
        p = jax.nn.softmax(s, axis=-1)
        return jnp.einsum('bhqk,bkhd->bqhd', p, vf)

    out = lax.map(one_block, xs)
    return out.transpose(1, 0, 2, 3, 4).reshape(B, S, H, dv).astype(v.dtype)


def hybrid_mixer(xn, positions, w_in, b_f, q_norm, w_uq, kv_norm, w_ukv, fox_out_norm, mla_out_norm, w_o):
    B, S, _ = xn.shape
    proj = xn @ w_in
    offs = [FOX_WIDTH, 2 * FOX_WIDTH, 3 * FOX_WIDTH, 3 * FOX_WIDTH + FOX_HEADS,
            3 * FOX_WIDTH + FOX_HEADS + Q_LORA_RANK,
            3 * FOX_WIDTH + FOX_HEADS + Q_LORA_RANK + KV_LORA_RANK]
    fq, fk, fv, f_logit, c_q, c_kv, k_rope = jnp.split(proj, offs, axis=-1)

    log_f = jax.nn.log_sigmoid(f_logit.astype(jnp.float32) + b_f.astype(jnp.float32))
    log_f_cum = jnp.cumsum(log_f, axis=1)
    fox = causal_block_attention(fq.reshape(B, S, FOX_HEADS, FOX_HEAD_DIM),
                                 fk.reshape(B, S, FOX_HEADS, FOX_HEAD_DIM),
                                 fv.reshape(B, S, FOX_HEADS, FOX_HEAD_DIM), log_f_cum)
    fox = rms_norm(fox, fox_out_norm.reshape(FOX_HEADS, FOX_HEAD_DIM)).reshape(B, S, FOX_WIDTH)

    q = (rms_norm(c_q, q_norm) @ w_uq).reshape(B, S, MLA_HEADS, MLA_NOPE_DIM + MLA_ROPE_DIM)
    q = jnp.concatenate([q[..., :MLA_NOPE_DIM], rotary(q[..., MLA_NOPE_DIM:], positions)], axis=-1)
    kv = (rms_norm(c_kv, kv_norm) @ w_ukv).reshape(B, S, MLA_HEADS, MLA_NOPE_DIM + MLA_V_DIM)
    k_nope, v = kv[..., :MLA_NOPE_DIM], kv[..., MLA_NOPE_DIM:]
    k_pe = rotary(k_rope, positions)
    k = jnp.concatenate([k_nope, jnp.broadcast_to(k_pe[:, :, None, :], (B, S, MLA_HEADS, MLA_ROPE_DIM))], axis=-1)
    mla = causal_block_attention(q, k, v)
    mla = rms_norm(mla, mla_out_norm.reshape(MLA_HEADS, MLA_V_DIM)).reshape(B, S, MLA_WIDTH)

    return jnp.concatenate([fox, mla], axis=-1) @ w_o


def hierarchical_moe(xn, w_group, w_router, w_gate, w_up, w_down):
    B, S, D = xn.shape
    T = B * S
    xt = xn.reshape(T, D)
    group_p = jax.nn.softmax((xt @ w_group).astype(jnp.float32), axis=-1)
    group_w, group_id = lax.top_k(group_p, 1)
    expert_logits = (xt @ w_router).astype(jnp.float32).reshape(T, N_GROUPS, EXPERTS_PER_GROUP)
    in_group = jnp.take_along_axis(expert_logits, group_id[:, :, None], axis=1)[:, 0]
    top_logits, local_id = lax.top_k(in_group, TOP_K_IN_GROUP)
    gates = group_w * jax.nn.softmax(top_logits, axis=-1)
    expert_id = group_id * EXPERTS_PER_GROUP + local_id

    A = T * TOP_K_IN_GROUP
    e_flat = expert_id.reshape(A)
    tok_flat = jnp.repeat(jnp.arange(T), TOP_K_IN_GROUP)
    g_flat = gates.reshape(A)
    order = jnp.argsort(e_flat)
    e_sorted, tok_sorted, g_sorted = e_flat[order], tok_flat[order], g_flat[order]
    counts = jnp.bincount(e_flat, length=N_EXPERTS)
    padded = (counts + EXPERT_BLOCK - 1) // EXPERT_BLOCK * EXPERT_BLOCK
    start = jnp.cumsum(counts) - counts
    pend = jnp.cumsum(padded)
    pstart = pend - padded
    dest = pstart[e_sorted] + (jnp.arange(A) - start[e_sorted])
    n_blocks = (A + N_EXPERTS * (EXPERT_BLOCK - 1) + EXPERT_BLOCK - 1) // EXPERT_BLOCK
    buf = jnp.zeros((n_blocks * EXPERT_BLOCK, D), xn.dtype).at[dest].set(xt[tok_sorted])
    block_expert = jnp.clip(jnp.searchsorted(pend, jnp.arange(n_blocks) * EXPERT_BLOCK, side='right'), 0, N_EXPERTS - 1)

    def expert_block(args):
        xb, e = args
        h = jax.nn.silu(xb @ w_gate[e]) * (xb @ w_up[e])
        return h @ w_down[e]

    ybuf = lax.map(expert_block, (buf.reshape(n_blocks, EXPERT_BLOCK, D), block_expert)).reshape(-1, D)
    contrib = ybuf[dest].astype(jnp.float32) * g_sorted[:, None]
    y = jax.ops.segment_sum(contrib, tok_sorted, num_segments=T)
    return y.reshape(B, S, D).astype(xn.dtype)


def setup_inputs(seed: int = 0) -> dict:
    key = jax.random.key(seed)
    ks = jax.random.split(key, 20)
    f32 = jnp.float32

    def dense(k, shape, fan_in):
        return jax.random.normal(k, shape, f32) * (fan_in ** -0.5)

    def gain(k, shape):
        return 1.0 + 0.02 * jax.random.normal(k, shape, f32)

    return {
        'x': jax.random.normal(ks[0], (BATCH, SEQ, D_MODEL), f32),
        'positions': jnp.broadcast_to(jnp.arange(SEQ, dtype=jnp.int32), (BATCH, SEQ)),
        'attn_norm': gain(ks[1], (DEPTH, D_MODEL)),
        'w_in': dense(ks[2], (DEPTH, D_MODEL, IN_WIDTH), D_MODEL),
        'b_f': jax.random.uniform(ks[3], (DEPTH, FOX_HEADS), f32, 1.0, 4.0),
        'q_norm': gain(ks[4], (DEPTH, Q_LORA_RANK)),
        'w_uq': dense(ks[5], (DEPTH, Q_LORA_RANK, MLA_HEADS * (MLA_NOPE_DIM + MLA_ROPE_DIM)), Q_LORA_RANK),
        'kv_norm': gain(ks[6], (DEPTH, KV_LORA_RANK)),
        'w_ukv': dense(ks[7], (DEPTH, KV_LORA_RANK, MLA_HEADS * (MLA_NOPE_DIM + MLA_V_DIM)), KV_LORA_RANK),
        'fox_out_norm': gain(ks[8], (DEPTH, FOX_WIDTH)),
        'mla_out_norm': gain(ks[9], (DEPTH, MLA_WIDTH)),
        'w_o': dense(ks[10], (DEPTH, D_MIX, D_MODEL), D_MIX),
        'ffn_norm': gain(ks[11], (DEPTH, D_MODEL)),
        'w_group': dense(ks[12], (DEPTH, D_MODEL, N_GROUPS), D_MODEL),
        'w_router': dense(ks[13], (DEPTH, D_MODEL, N_EXPERTS), D_MODEL),
        'w_gate': dense(ks[14], (DEPTH, N_EXPERTS, D_MODEL, D_EXPERT), D_MODEL),
        'w_up': dense(ks[15], (DEPTH, N_EXPERTS, D_MODEL, D_EXPERT), D_MODEL),
        'w_down': dense(ks[16], (DEPTH, N_EXPERTS, D_EXPERT, D_MODEL), D_EXPERT),
        'final_norm': gain(ks[17], (D_MODEL,)),
    }


def reference(x, positions, attn_norm, w_in, b_f, q_norm, w_uq, kv_norm, w_ukv, fox_out_norm, mla_out_norm, w_o,
              ffn_norm, w_group, w_router, w_gate, w_up, w_down, final_norm):
    h = x
    for l in range(DEPTH):
        h = h + hybrid_mixer(rms_norm(h, attn_norm[l]), positions, w_in[l], b_f[l], q_norm[l], w_uq[l],
                             kv_norm[l], w_ukv[l], fox_out_norm[l], mla_out_norm[l], w_o[l])
        h = h + hierarchical_moe(rms_norm(h, ffn_norm[l]), w_group[l], w_router[l], w_gate[l], w_up[l], w_down[l])
    return rms_norm(h, final_norm)
```

```python
import math
from contextlib import ExitStack

import numpy as np
import ml_dtypes
import concourse.bass as bass
import concourse.mybir as mybir
from concourse.bass_utils import run_bass_kernel_spmd

F32, BF16, I32 = mybir.dt.float32, mybir.dt.bfloat16, mybir.dt.int32
AF = mybir.ActivationFunctionType
ALU = mybir.AluOpType
AX = mybir.AxisListType

S = 4096
D = 1024
L = 2
NT = S // 128
NCH = S // 512
INW = 1960
NBLK = 96
NROWS = NBLK * 128
EPS = 1e-6
TWO_PI = 2.0 * math.pi


class TK:
    def __init__(self, nc, es):
        self.nc = nc
        self.E = dict(pe=nc.tensor, act=nc.scalar, dve=nc.vector, pool=nc.gpsimd, sp=nc.sync)
        self.sem = {k: es.enter_context(nc.semaphore("sem_" + k)) for k in self.E}
        self.cnt = {k: 0 for k in self.E}
        self.seen = {k: {} for k in self.E}
        self.lastw = {}
        self.readers = {}
        self.NS = 40
        self.dsem = [es.enter_context(nc.semaphore("dsem%d" % i)) for i in range(self.NS)]
        self.dma_i = 0

    def _semof(self, src):
        return self.dsem[src[1]] if isinstance(src, tuple) else self.sem[src]

    def _wait(self, e, tok):
        src, val = tok
        if src == "pe" and e == "pe":
            return
        if self.seen[e].get(src, 0) >= val:
            return
        self.E[e].wait_ge(self._semof(src), val)
        self.seen[e][src] = val

    def _deps(self, e, reads, writes):
        for r in reads:
            t = self.lastw.get(r)
            if t:
                self._wait(e, t)
        for w in writes:
            t = self.lastw.get(w)
            if t:
                self._wait(e, t)
            for src, val in list(self.readers.get(w, {}).items()):
                self._wait(e, (src, val))

    def _commit(self, tok, reads, writes):
        for r in reads:
            d = self.readers.setdefault(r, {})
            d[tok[0]] = max(d.get(tok[0], 0), tok[1])
        for w in writes:
            self.lastw[w] = tok
            self.readers[w] = {}

    PSUM_T = {"ptrA", "paccA", "pS", "pO", "py", "ptx", "pgu", "pht", "pyd"}
    PSUM_S = {"pfl", "pdt", "pSS", "plg"}

    def _is_psum(self, r):
        return (isinstance(r, tuple) and r[0] in self.PSUM_T) or (isinstance(r, str) and r in self.PSUM_S)

    def op(self, e, fn, reads=(), writes=()):
        px = [r for r in reads if self._is_psum(r)]
        if px:
            reads = [r for r in reads if not self._is_psum(r)]
            writes = list(writes) + [r for r in px if r not in writes]
        self._deps(e, reads, writes)
        ins = fn(self.E[e])
        self.cnt[e] += 1
        ins.then_inc(self.sem[e], 1)
        tok = (e, self.cnt[e])
        self._commit(tok, reads, writes)
        return tok

    def dma(self, e, fn, reads=(), writes=()):
        self._deps(e, reads, writes)
        i = self.dma_i
        self.dma_i += 1
        slot, k = i % self.NS, i // self.NS
        src = ("d", slot)
        if k > 0:
            self._wait(e, (src, 16 * k))
        ins = fn(self.E[e])
        ins.then_inc(self.dsem[slot], 16)
        tok = (src, 16 * (k + 1))
        self._commit(tok, reads, writes)
        return tok

    def barrier(self):
        for e in ("pe", "act", "dve", "pool", "sp"):
            for t in list(self.lastw.values()):
                self._wait(e, t)
            for d in list(self.readers.values()):
                for src, val in list(d.items()):
                    self._wait(e, (src, val))

    def wait_all(self, e, resources):
        for r in resources:
            t = self.lastw.get(r)
            if t:
                self._wait(e, t)


def build_program(debug=False, stop=None):
    nc = bass.Bass("TRN2", target_bir_lowering=False)
    try:
        nc.allow_low_precision("bf16 matmul operands with fp32 accumulation")
    except Exception:
        pass
    try:
        nc.allow_non_contiguous_dma("small strided layout DMAs")
    except Exception:
        pass

    def din(name, shape, dt=F32):
        return nc.dram_tensor(name, list(shape), dt, kind="ExternalInput").ap()

    def dscr(name, shape, dt):
        return nc.dram_tensor(name, list(shape), dt, kind="ExternalOutput" if debug else "Internal").ap()

    x_in = din("x", [S, D])
    pos_in = din("pos", [128, S], I32)
    gA_in = din("gA", [L, 128, 8])
    gF_in = din("gF", [L, 128, 8])
    gQ_in = din("gQ", [L, 128, 2])
    gKV_in = din("gKV", [L, 128, 1])
    gO_in = din("gO", [L, 128, 8])
    bf_in = din("bfb", [L, 128, 32])
    fin_in = din("fing", [128, D])
    win_in = din("win", [L, 128, 8, INW + 32])
    wuq_in = din("wuq", [L, 128, 2, 768 + 256])
    wukv_in = din("wukv", [L, 128, 1024])
    wo_in = din("wo", [L, 128, 8, 1024])
    wrt_in = din("wrt", [L, 128, 8, 40])
    wgu_in = [din("wgu%d" % l, [4096, 8 * 512]) for l in range(L)]
    wd_in = [din("wd%d" % l, [4096, 2 * 1024]) for l in range(L)]
    cst_in = din("cst", [128, 6, 128])
    cst2_in = din("cst2", [128, 4 + NBLK])
    y_out = nc.dram_tensor("y", [S, D], F32, kind="ExternalOutput").ap()

    hA = dscr("hA", [S, D], F32)
    hB = dscr("hB", [S, D], F32)
    QF = dscr("QF", [8, 67, S], BF16)
    KF = dscr("KF", [8, 64, S], BF16)
    VF = dscr("VF", [8, 128, NT, 65], BF16)
    QM = dscr("QM", [8, 96, S], BF16)
    KM = dscr("KM", [8, 96, S], BF16)
    VM = dscr("VM", [8, 128, NT, 65], BF16)
    AT = dscr("AT", [16, 64, S], BF16)
    XB = dscr("XB", [NROWS, D], BF16)
    YB = dscr("YB", [NROWS, D], F32)

    es = ExitStack()
    tk = TK(nc, es)

    uid = [0]

    def sb(es_, name, shape, dt):
        uid[0] += 1
        return es_.enter_context(nc.sbuf_tensor("s%d_%s" % (uid[0], name), list(shape), dt))

    def ps(es_, name, shape, dt=F32):
        uid[0] += 1
        return es_.enter_context(nc.psum_tensor("p%d_%s" % (uid[0], name), list(shape), dt))

    cst = sb(es, "cst", [128, 6, 128], F32)
    cst2 = sb(es, "cst2", [128, 4 + NBLK], F32)
    identb = sb(es, "identb", [128, 128], BF16)
    maskb = sb(es, "maskb", [128, 128], BF16)
    NDp_all = [sb(es, "NDp%d" % i, [128, NT, 8], F32) for i in range(L)]
    tk.dma("sp", lambda q: q.dma_start(out=cst[:], in_=cst_in[:, :, :]), writes=["cst"])
    tk.dma("sp", lambda q: q.dma_start(out=cst2[:], in_=cst2_in[:, :]), writes=["cst2"])
    identf = cst[:, 0, :]
    Uincl = cst[:, 1, :]
    Ustrict = cst[:, 2, :]
    onesf = cst[:, 3, :]
    wselb = sb(es, "wselb", [65, 64], BF16)
    tk.op("dve", lambda v: v.tensor_copy(out=identb[:], in_=cst[:, 0, :]), reads=["cst"], writes=["identb"])
    tk.op("dve", lambda v: v.tensor_copy(out=maskb[:], in_=cst[:, 4, :]), reads=["cst"], writes=["maskb"])
    tk.op("dve", lambda v: v.tensor_copy(out=wselb[:], in_=cst[0:65, 5, 0:64]), reads=["cst"], writes=["wselb"])

    class _Stop(Exception):
        pass

    def chk(name):
        if stop == name:
            tk.barrier()
            raise _Stop()

    try:
      for l in range(L):
          hsrc = x_in if l == 0 else (hA if l % 2 == 1 else hB)
          hcur = hA if l % 2 == 0 else hB
          last = (l == L - 1)
          with ExitStack() as pa:
              win_bf = sb(pa, "win_bf", [128, 8, INW + 32], BF16)
              wqn_bf = sb(pa, "wqn_bf", [128, 2, 512], BF16)
              wqr_bf = sb(pa, "wqr_bf", [128, 2, 256], BF16)
              wqt_bf = sb(pa, "wqt_bf", [128, 2, 256], BF16)
              wukv_bf = sb(pa, "wukv_bf", [128, 1024], BF16)
              wkn_bf = sb(pa, "wkn_bf", [128, 512], BF16)
              gA = sb(pa, "gA", [128, 8], F32)
              posi = sb(pa, "posi", [128, 512], I32)
              gQ = sb(pa, "gQ", [128, 2], F32)
              gKV = sb(pa, "gKV", [128, 1], F32)
              bfb = sb(pa, "bfb", [128, 32], F32)
              wstg = [sb(pa, "wstg%d" % i, [128, INW + 32], F32) for i in range(2)]
              tk.dma("sp", lambda q: q.dma_start(out=gA[:], in_=gA_in[l]), writes=["gA"])
              tk.dma("sp", lambda q: q.dma_start(out=gQ[:], in_=gQ_in[l]), writes=["gQ"])
              tk.dma("sp", lambda q: q.dma_start(out=gKV[:], in_=gKV_in[l]), writes=["gKV"])
              tk.dma("sp", lambda q: q.dma_start(out=bfb[:], in_=bf_in[l]), writes=["bfb"])
              for k in range(8):
                  st = wstg[k % 2]
                  rs = ("wstg", k % 2)
                  tk.dma("sp", lambda q: q.dma_start(out=st[:], in_=win_in[l, :, k, :]), writes=[rs])
                  tk.op("dve", lambda v: v.tensor_scalar(out=win_bf[:, k, 0:512], in0=st[:, 0:512], scalar1=gA[:, k:k + 1],
                                                         scalar2=0.125, op0=ALU.mult, op1=ALU.mult),
                        reads=[rs, "gA"], writes=["win_bf"])
                  tk.op("dve", lambda v: v.tensor_scalar(out=win_bf[:, k, 512:INW], in0=st[:, 512:INW],
                                                         scalar1=gA[:, k:k + 1], scalar2=None, op0=ALU.mult),
                        reads=[rs, "gA"], writes=["win_bf"])
                  tk.op("dve", lambda v: v.tensor_scalar(out=win_bf[:, k, INW:INW + 16], in0=st[:, INW:INW + 16],
                                                         scalar1=gA[:, k:k + 1], scalar2=-1.0, op0=ALU.mult, op1=ALU.mult),
                        reads=[rs, "gA"], writes=["win_bf"])
                  tk.op("dve", lambda v: v.tensor_scalar(out=win_bf[:, k, INW + 16:INW + 32], in0=st[:, INW + 16:INW + 32],
                                                         scalar1=gA[:, k:k + 1], scalar2=None, op0=ALU.mult),
                        reads=[rs, "gA"], writes=["win_bf"])
              qs = 96.0 ** -0.5
              for kk in range(2):
                  st = wstg[kk % 2]
                  rs = ("wstg", kk % 2)
                  tk.dma("sp", lambda q: q.dma_start(out=st[:, 0:1024], in_=wuq_in[l, :, kk, :]), writes=[rs])
                  sv = st[:, 0:768].rearrange("p (h c) -> p h c", c=96)
                  tk.op("dve", lambda v: v.tensor_scalar(out=wqn_bf[:, kk, :].rearrange("p (h c) -> p h c", c=64), in0=sv[:, :, 0:64],
                                                         scalar1=gQ[:, kk:kk + 1], scalar2=qs, op0=ALU.mult, op1=ALU.mult),
                        reads=[rs, "gQ"], writes=["wuq_bf"])
                  tk.op("dve", lambda v: v.tensor_scalar(out=wqr_bf[:, kk, :].rearrange("p (h c) -> p h c", c=32), in0=sv[:, :, 64:96],
                                                         scalar1=gQ[:, kk:kk + 1], scalar2=qs, op0=ALU.mult, op1=ALU.mult),
                        reads=[rs, "gQ"], writes=["wuq_bf"])
                  rv = st[:, 768:1024].rearrange("p (h c) -> p h c", c=32)
                  wt = wqt_bf[:, kk, :].rearrange("p (h c) -> p h c", c=32)
                  tk.op("dve", lambda v: v.tensor_scalar(out=wt[:, :, 0:16], in0=rv[:, :, 0:16], scalar1=gQ[:, kk:kk + 1],
                                                         scalar2=-qs, op0=ALU.mult, op1=ALU.mult),
                        reads=[rs, "gQ"], writes=["wuqr_bf"])
                  tk.op("dve", lambda v: v.tensor_scalar(out=wt[:, :, 16:32], in0=rv[:, :, 16:32], scalar1=gQ[:, kk:kk + 1],
                                                         scalar2=qs, op0=ALU.mult, op1=ALU.mult),
                        reads=[rs, "gQ", "wuqr_bf"], writes=["wuqr_bf"])
              st = wstg[0]
              tk.dma("sp", lambda q: q.dma_start(out=st[:, 0:1024], in_=wukv_in[l]), writes=[("wstg", 0)])
              tk.op("dve", lambda v: v.tensor_scalar(out=wkn_bf[:].rearrange("p (h c) -> p h c", c=64),
                                                     in0=st[:, 0:1024].rearrange("p (h c) -> p h c", c=128)[:, :, 0:64],
                                                     scalar1=gKV[:, 0:1], scalar2=None, op0=ALU.mult), reads=[("wstg", 0), "gKV"], writes=["wukv_bf"])
              tk.op("dve", lambda v: v.tensor_scalar(out=wukv_bf[:], in0=st[:, 0:1024], scalar1=gKV[:, 0:1], scalar2=None,
                                                     op0=ALU.mult), reads=[("wstg", 0), "gKV"], writes=["wukv_bf"])
              W_ALL = ["win_bf", "wuq_bf", "wuqr_bf", "wukv_bf"]
              chk("A0")

              NDtok = NDp_all[l]
              hxs = [sb(pa, "hx%d" % i, [128, 4, D], F32) for i in range(2)]
              junk = sb(pa, "junk", [128, D], F32)
              ss = sb(pa, "ss", [128, 4], F32)
              rstd = sb(pa, "rstd", [128, 4], F32)
              xns = [sb(pa, "xn%d" % i, [128, 4, D], BF16) for i in range(2)]
              xnTs = [sb(pa, "xnT%d" % i, [128, 8, 512], BF16) for i in range(2)]
              stg = [sb(pa, "stg%d" % i, [128, 512], BF16) for i in range(4)]
              vstf = sb(pa, "vstf", [128, 4, 8, 65], BF16)
              vstm = sb(pa, "vstm", [128, 4, 8, 65], BF16)
              zf = sb(pa, "zf", [128, 32], F32)
              nl = sb(pa, "nl", [128, 32], F32)
              SX = [sb(pa, "SX%d" % i, [128, 5, 8], F32) for i in range(2)]
              dq = sb(pa, "dq", [8, 3, 512], BF16)
              dr = [sb(pa, "dr%d" % i, [8, 512], F32) for i in range(2)]
              cq32 = sb(pa, "cq32", [128, 3, 512], F32)
              sq32 = sb(pa, "sq32", [128, 3, 512], F32)
              rsq = sb(pa, "rsq", [128, 2, 512], F32)
              cqn = sb(pa, "cqn", [128, 3, 512], BF16)
              posf = sb(pa, "posf", [128, 512], F32)
              rr = sb(pa, "rr", [128, 2, 512], F32)
              ri = sb(pa, "ri", [128, 2, 512], I32)
              rf = sb(pa, "rf", [128, 2, 512], F32)
              cs = sb(pa, "cs", [128, 2, 512], F32)
              t1 = sb(pa, "t1", [128, 512], F32)
              t2 = sb(pa, "t2", [128, 512], F32)
              kpe = sb(pa, "kpe", [32, 512], BF16)
              ptr = [ps(pa, "ptrA%d" % i, [128, 512], BF16) for i in range(2)]
              pacc = [ps(pa, "paccA%d" % i, [128, 512], F32) for i in range(4)]
              pfl = ps(pa, "pflA", [128, 512], F32)
              pdt = ps(pa, "pdtA", [128, 512], F32)

              tk.op("pool", lambda v: v.memset(vstf[:], 1.0), writes=["vstf"])
              tk.op("pool", lambda v: v.memset(vstm[:], 1.0), writes=["vstm"])
              tk.op("pool", lambda v: v.memset(SX[0][:], 0.0), writes=[("SX", 0)])

              acc_i = [0]

              def next_acc():
                  i = acc_i[0] % 4
                  acc_i[0] += 1
                  return pacc[i], ("paccA", i)

              stg_i = [0]

              def next_stg():
                  i = stg_i[0] % 4
                  stg_i[0] += 1
                  return stg[i], ("stg", i)

              ev_i = [0]

              def evac(out_ap, in_ap, reads, writes):
                  ev_i[0] += 1
                  if ev_i[0] % 2 == 0:
                      return tk.op("act", lambda a: a.copy(out=out_ap, in_=in_ap), reads=reads, writes=writes)
                  return tk.op("dve", lambda v: v.tensor_copy(out=out_ap, in_=in_ap), reads=reads, writes=writes)

              def pro_load(c):
                  hx = hxs[c % 2]
                  HX = ("hx", c % 2)
                  cs0 = c * 512
                  tk.dma("sp", lambda q: q.dma_start(out=hx[:], in_=hsrc[cs0:cs0 + 512, :].rearrange("(t p) d -> p t d", p=128)),
                         reads=[("hB", l, 4 * c + t_) for t_ in range(4)], writes=[HX])

              def prologue(c, parts=range(9)):
                  hx, xn, xnT = hxs[c % 2], xns[c % 2], xnTs[c % 2]
                  HX, XNr, XNT_ = ("hx", c % 2), ("xn", c % 2), ("xnT", c % 2)
                  for part in parts:
                      if part < 4:
                          t = part
                          tk.op("act", lambda a: a.activation(out=junk[:], in_=hx[:, t, :], func=AF.Square), reads=[HX], writes=["junk"])
                          tk.op("dve", lambda v: v.tensor_reduce(out=ss[:, t:t + 1], in_=junk[:], axis=AX.X, op=ALU.add),
                                reads=["junk"], writes=["ss"])
                      elif part == 4:
                          tk.op("act", lambda a: a.activation(out=rstd[:], in_=ss[:], func=AF.Ln, bias=EPS, scale=1.0 / D),
                                reads=["ss"], writes=["rstd"])
                          tk.op("act", lambda a: a.activation(out=rstd[:], in_=rstd[:], func=AF.Exp, scale=-0.5), reads=["rstd"], writes=["rstd"])
                      else:
                          t = part - 5
                          tk.op("dve", lambda v: v.tensor_scalar(out=xn[:, t, :], in0=hx[:, t, :], scalar1=rstd[:, t:t + 1], scalar2=None,
                                                                 op0=ALU.mult), reads=[HX, "rstd"], writes=[XNr])

              def prologue2(c):
                  xn, xnT = xns[c % 2], xnTs[c % 2]
                  XNr, XNT_ = ("xn", c % 2), ("xnT", c % 2)
                  for k in range(8):
                      pt, ptn = ptr[k % 2], ("ptrA", k % 2)
                      for t in range(4):
                          tk.op("pe", lambda p: p.transpose(out=pt[:, t * 128:(t + 1) * 128], in_=xn[:, t, k * 128:(k + 1) * 128],
                                                           identity=identb[:]), reads=[XNr, "identb"], writes=[ptn])
                      evac(xnT[:, k, :], pt[:, :], [ptn], [XNT_])


              def body(c):
                  cs0 = c * 512
                  csl = slice(cs0, cs0 + 512)
                  xnT = xnTs[c % 2]
                  XNT_ = ("xnT", c % 2)
                  chk("A2")
                  for t in range(4):
                      acc, accn = next_acc()
                      for k in range(8):
                          tk.op("pe", lambda p: p.matmul(acc[:, :], lhsT=xnT[:, k, t * 128:(t + 1) * 128], rhs=win_bf[:, k, 1024:1536],
                                                         start=(k == 0), stop=(k == 7)), reads=[XNT_] + W_ALL, writes=[accn])
                      evac(vstf[:, t, :, 0:64], acc[:, :].rearrange("p (h e) -> p h e", e=64), [accn], ["vstf"])
                      for k in range(8):
                          tk.op("pe", lambda p: p.matmul(pfl[:, t * 8:(t + 1) * 8], lhsT=xnT[:, k, t * 128:(t + 1) * 128],
                                                         rhs=win_bf[:, k, 1536:1544], start=(k == 0), stop=(k == 7)),
                                reads=[XNT_] + W_ALL, writes=["pfl"])
                  for h in range(8):
                      tk.dma("pool", lambda q: q.dma_start(out=VF[h, :, 4 * c:4 * c + 4, :], in_=vstf[:, :, h, :]),
                             reads=["vstf"], writes=[("VF", h, c)])
                  chk("A3")
                  tk.op("dve", lambda v: v.tensor_tensor(out=zf[:], in0=pfl[:, 0:32], in1=bfb[:], op=ALU.add),
                        reads=["pfl", "bfb"], writes=["zf"])
                  tk.op("act", lambda a: a.activation(out=zf[:], in_=zf[:], func=AF.Exp, scale=-1.0), reads=["zf"], writes=["zf"])
                  tk.op("act", lambda a: a.activation(out=nl[:], in_=zf[:], func=AF.Ln, bias=1.0), reads=["zf"], writes=["nl"])
                  sxc, sxn = SX[c % 2], SX[(c + 1) % 2]
                  for t in range(4):
                      tk.op("dve", lambda v: v.tensor_tensor(out=sxc[:, t + 1, :], in0=sxc[:, t, :], in1=nl[:, t * 8:(t + 1) * 8], op=ALU.add),
                            reads=["nl", ("SX", c % 2)], writes=[("SX", c % 2)])
                  tk.op("dve", lambda v: v.tensor_copy(out=sxn[:, 0, :], in_=sxc[:, 4, :]), reads=[("SX", c % 2)], writes=[("SX", (c + 1) % 2)])
                  chk("A4")
                  for m in range(3):
                      acc, accn = next_acc()
                      col0 = 1544 + m * 128
                      for k in range(8):
                          tk.op("pe", lambda p: p.matmul(acc[:, :], lhsT=win_bf[:, k, col0:col0 + 128], rhs=xnT[:, k, :],
                                                         start=(k == 0), stop=(k == 7)), reads=[XNT_] + W_ALL, writes=[accn])
                      tk.op("act", lambda a: a.activation(out=sq32[:, m, :], in_=acc[:, :], func=AF.Square), reads=[accn], writes=["sq32"])
                      tk.op("dve", lambda v: v.tensor_copy(out=cq32[:, m, :], in_=acc[:, :]), reads=[accn, "sq32"], writes=["cq32"])
                  chk("A4a")
                  acc, accn = next_acc()
                  tk.op("pe", lambda p: p.matmul(acc[:, :], lhsT=onesf, rhs=sq32[:, 0, :], start=True, stop=False), reads=["sq32", "cst"], writes=[accn])
                  tk.op("pe", lambda p: p.matmul(acc[:, :], lhsT=onesf, rhs=sq32[:, 1, :], start=False, stop=True), reads=["sq32", "cst"], writes=[accn])
                  tk.op("act", lambda a: a.activation(out=rsq[:, 0, :], in_=acc[:, :], func=AF.Ln, bias=EPS, scale=1.0 / 256), reads=[accn], writes=["rsq"])
                  acc, accn = next_acc()
                  tk.op("pe", lambda p: p.matmul(acc[:, :], lhsT=onesf, rhs=sq32[:, 2, :], start=True, stop=True), reads=["sq32", "cst"], writes=[accn])
                  tk.op("act", lambda a: a.activation(out=rsq[:, 1, :], in_=acc[:, :], func=AF.Ln, bias=EPS, scale=1.0 / 128), reads=[accn], writes=["rsq"])
                  chk("A4b")
                  tk.op("act", lambda a: a.activation(out=rsq[:], in_=rsq[:], func=AF.Exp, scale=-0.5), reads=["rsq"], writes=["rsq"])
                  chk("A4c")
                  for m in range(3):
                      tk.op("dve", lambda v: v.tensor_tensor(out=cqn[:, m, :], in0=cq32[:, m, :], in1=rsq[:, 0 if m < 2 else 1, :], op=ALU.mult),
                            reads=["cq32", "rsq"], writes=["cqn"])
                  chk("A5")
                  tk.dma("sp", lambda q: q.dma_start(out=posi[:], in_=pos_in[:, csl]), writes=["posi"])
                  tk.op("dve", lambda v: v.tensor_copy(out=posf[:], in_=posi[:]), reads=["posi"], writes=["posf"])
                  tk.op("dve", lambda v: v.tensor_scalar(out=rr[:, 0, :], in0=posf[:], scalar1=cst2[:, 0:1], scalar2=None, op0=ALU.mult),
                        reads=["posf", "cst2"], writes=["rr"])
                  tk.op("dve", lambda v: v.tensor_scalar(out=rr[:, 1, :], in0=rr[:, 0, :], scalar1=0.25, scalar2=None, op0=ALU.add),
                        reads=["rr"], writes=["rr"])
                  tk.op("dve", lambda v: v.tensor_copy(out=ri[:], in_=rr[:]), reads=["rr"], writes=["ri"])
                  tk.op("dve", lambda v: v.tensor_copy(out=rf[:], in_=ri[:]), reads=["ri"], writes=["rf"])
                  tk.op("dve", lambda v: v.tensor_tensor(out=rr[:], in0=rr[:], in1=rf[:], op=ALU.subtract), reads=["rr", "rf"], writes=["rr"])
                  tk.op("act", lambda a: a.activation(out=cs[:], in_=rr[:], func=AF.Sin, scale=TWO_PI * (1.0 - 2e-6)), reads=["rr"], writes=["cs"])
                  chk("A6")
                  acc, accn = next_acc()
                  acc2, accn2 = next_acc()
                  for k in range(8):
                      tk.op("pe", lambda p: p.matmul(acc[0:32, :], lhsT=win_bf[:, k, 1928:1960], rhs=xnT[:, k, :], start=(k == 0), stop=(k == 7)),
                            reads=[XNT_] + W_ALL, writes=[accn])
                  for k in range(8):
                      tk.op("pe", lambda p: p.matmul(acc2[0:32, :], lhsT=win_bf[:, k, INW:INW + 32], rhs=xnT[:, k, :], start=(k == 0), stop=(k == 7)),
                            reads=[XNT_] + W_ALL, writes=[accn2])
                  tk.op("dve", lambda v: v.tensor_tensor(out=t1[0:32, :], in0=acc[0:32, :], in1=cs[0:32, 1, :], op=ALU.mult), reads=[accn, "cs"], writes=["t1"])
                  tk.op("dve", lambda v: v.tensor_tensor(out=t2[0:32, :], in0=acc2[0:32, :], in1=cs[0:32, 0, :], op=ALU.mult), reads=[accn2, "cs"], writes=["t2"])
                  tk.op("dve", lambda v: v.tensor_tensor(out=kpe[:], in0=t1[0:32, :], in1=t2[0:32, :], op=ALU.add), reads=["t1", "t2"], writes=["kpe"])
                  for h in range(8):
                      tk.dma("pool", lambda q: q.dma_start(out=KM[h, 64:96, csl], in_=kpe[:]), reads=["kpe"], writes=[("KMR", h, c)])
                  sxc = SX[c % 2]
                  SXN = ("SX", c % 2)
                  for t in range(4):
                      nlt = nl[:, t * 8:(t + 1) * 8]
                      tk.op("pe", lambda p: p.matmul(pfl[:, 64 + t * 8:72 + t * 8],
                                                     lhsT=Uincl, rhs=nlt, start=True, stop=False), reads=["nl", "cst"], writes=["pfl"])
                      tk.op("pe", lambda p: p.matmul(pfl[:, 64 + t * 8:72 + t * 8], lhsT=onesf, rhs=sxc[:, t, :], start=False, stop=True),
                            reads=[SXN, "cst"], writes=["pfl"])
                      tk.op("pe", lambda p: p.matmul(pdt[0:8, t * 128:(t + 1) * 128], lhsT=nlt, rhs=Uincl, start=True, stop=False),
                            reads=["nl", "cst"], writes=["pdt"])
                      tk.op("pe", lambda p: p.matmul(pdt[0:8, t * 128:(t + 1) * 128], lhsT=sxc[:, t, :], rhs=onesf, start=False, stop=True),
                            reads=[SXN, "cst"], writes=["pdt"])
                  tk.op("dve", lambda v: v.tensor_copy(out=NDtok[:, 4 * c:4 * c + 4, :], in_=pfl[:, 64:96].rearrange("p (t h) -> p t h", h=8)),
                        reads=["pfl"], writes=["NDp"])
                  tk.op("dve", lambda v: v.tensor_scalar(out=dq[:, 0, :], in0=pdt[0:8, :], scalar1=-1.0, scalar2=None, op0=ALU.mult),
                        reads=["pdt"], writes=["dq"])
                  tk.op("dve", lambda v: v.scalar_tensor_tensor(out=dr[0][:], in0=pdt[0:8, :], scalar=-1.0, in1=dq[:, 0, :],
                                                                op0=ALU.mult, op1=ALU.subtract), reads=["pdt", "dq"], writes=["dr0"])
                  tk.op("dve", lambda v: v.tensor_copy(out=dq[:, 1, :], in_=dr[0][:]), reads=["dr0"], writes=["dq"])
                  tk.op("dve", lambda v: v.tensor_tensor(out=dr[1][:], in0=dr[0][:], in1=dq[:, 1, :], op=ALU.subtract),
                        reads=["dr0", "dq"], writes=["dr1"])
                  tk.op("dve", lambda v: v.tensor_copy(out=dq[:, 2, :], in_=dr[1][:]), reads=["dr1"], writes=["dq"])
                  tk.dma("pool", lambda q: q.dma_start(out=QF[:, 64:67, csl], in_=dq[:]), reads=["dq"], writes=[("QFD", c)])

                  chk("A1")
                  for kind in range(2):
                      for m in range(4):
                          acc, accn = next_acc()
                          col0 = kind * 512 + m * 128
                          for k in range(8):
                              tk.op("pe", lambda p: p.matmul(acc[:, :], lhsT=win_bf[:, k, col0:col0 + 128], rhs=xnT[:, k, :],
                                                             start=(k == 0), stop=(k == 7)), reads=[XNT_] + W_ALL, writes=[accn])
                          sg, sgn = next_stg()
                          evac(sg[:], acc[:, :], [accn], [sgn])
                          dst = QF if kind == 0 else KF
                          for hh in range(2):
                              h = 2 * m + hh
                              tk.dma("sp", lambda q: q.dma_start(out=dst[h, 0:64, csl], in_=sg[hh * 64:(hh + 1) * 64, :]),
                                     reads=[sgn], writes=[("QKF", kind, h, c)])
                          if c + 1 < NCH:
                              g_ = kind * 4 + m
                              prologue(c + 1, parts={0: [0, 1], 1: [2, 3, 4], 2: [5], 3: [6], 4: [7], 5: [8]}.get(g_, []))
                  chk("A7")
                  for m in range(4):
                      acc, accn = next_acc()
                      for kk in range(2):
                          tk.op("pe", lambda p: p.matmul(acc[:, :], lhsT=wqn_bf[:, kk, m * 128:(m + 1) * 128], rhs=cqn[:, kk, :],
                                                         start=(kk == 0), stop=(kk == 1)), reads=["cqn"] + W_ALL, writes=[accn])
                      sg, sgn = next_stg()
                      evac(sg[:], acc[:, :], [accn], [sgn])
                      for hh in range(2):
                          h = 2 * m + hh
                          tk.dma("pool", lambda q: q.dma_start(out=QM[h, 0:64, csl], in_=sg[hh * 64:(hh + 1) * 64, :]),
                                 reads=[sgn], writes=[("QM", h, c)])
                  for r in range(2):
                      acc, accn = next_acc()
                      acc2, accn2 = next_acc()
                      for kk in range(2):
                          tk.op("pe", lambda p: p.matmul(acc[:, :], lhsT=wqr_bf[:, kk, r * 128:(r + 1) * 128], rhs=cqn[:, kk, :],
                                                         start=(kk == 0), stop=(kk == 1)), reads=["cqn"] + W_ALL, writes=[accn])
                      for kk in range(2):
                          tk.op("pe", lambda p: p.matmul(acc2[:, :], lhsT=wqt_bf[:, kk, r * 128:(r + 1) * 128], rhs=cqn[:, kk, :],
                                                         start=(kk == 0), stop=(kk == 1)), reads=["cqn"] + W_ALL, writes=[accn2])
                      sg, sgn = next_stg()
                      tk.op("dve", lambda v: v.tensor_tensor(out=t1[:], in0=acc[:, :], in1=cs[:, 1, :], op=ALU.mult), reads=[accn, "cs"], writes=["t1"])
                      tk.op("dve", lambda v: v.tensor_tensor(out=t2[:], in0=acc2[:, :], in1=cs[:, 0, :], op=ALU.mult), reads=[accn2, "cs"], writes=["t2"])
                      tk.op("dve", lambda v: v.tensor_tensor(out=sg[:], in0=t1[:], in1=t2[:], op=ALU.add), reads=["t1", "t2"], writes=[sgn])
                      for jj in range(4):
                          h = 4 * r + jj
                          tk.dma("pool", lambda q: q.dma_start(out=QM[h, 64:96, csl], in_=sg[jj * 32:(jj + 1) * 32, :]),
                                 reads=[sgn], writes=[("QMR", h, c)])
                  chk("A8")
                  for m in range(4):
                      acc, accn = next_acc()
                      tk.op("pe", lambda p: p.matmul(acc[:, :], lhsT=wkn_bf[:, m * 128:(m + 1) * 128], rhs=cqn[:, 2, :], start=True, stop=True),
                            reads=["cqn"] + W_ALL, writes=[accn])
                      sg, sgn = next_stg()
                      evac(sg[:], acc[:, :], [accn], [sgn])
                      for hh in range(2):
                          h = 2 * m + hh
                          tk.dma("pool", lambda q: q.dma_start(out=KM[h, 0:64, csl], in_=sg[hh * 64:(hh + 1) * 64, :]), reads=[sgn], writes=[("KMN", h, c)])
                  for t in range(4):
                      for half in range(2):
                          acc, accn = next_acc()
                          tk.op("pe", lambda p: p.matmul(acc[:, :], lhsT=cqn[:, 2, t * 128:(t + 1) * 128], rhs=wukv_bf[:, half * 512:(half + 1) * 512],
                                                         start=True, stop=True), reads=["cqn"] + W_ALL, writes=[accn])
                          evac(vstm[:, t, half * 4:(half + 1) * 4, 0:64], acc[:, :].rearrange("p (j e) -> p j e", e=128)[:, :, 64:128], [accn], ["vstm"])
                  for h in range(8):
                      tk.dma("pool", lambda q: q.dma_start(out=VM[h, :, 4 * c:4 * c + 4, :], in_=vstm[:, :, h, :]),
                             reads=["vstm"], writes=[("VM", h, c)])

              pro_load(0)
              prologue(0)
              prologue2(0)
              for c in range(NCH):
                  if c + 1 < NCH:
                      pro_load(c + 1)
                  body(c)
                  if c + 1 < NCH:
                      prologue2(c + 1)
              tk.barrier()

          if stop == "A":
              break
          with ExitStack() as pb:
              kt = [sb(pb, "kt%d" % i, [128, S], BF16) for i in range(2)]
              qt = [sb(pb, "qt%d" % i, [128, S], BF16) for i in range(2)]
              vt = [sb(pb, "vt%d" % i, [128, NT, 128], BF16) for i in range(2)]
              pT = [sb(pb, "pT%d" % i, [128, 512], BF16) for i in range(4)]
              num = [sb(pb, "num%d" % i, [65, 512], F32) for i in range(2)]
              sqns = [sb(pb, "sqn%d" % i, [65, 512], BF16) for i in range(2)]
              lnv = sb(pb, "lnv", [64, 512], F32)
              atb = [sb(pb, "atb%d" % i, [64, 512], BF16) for i in range(2)]
              pS = [ps(pb, "pS%d" % i, [128, 512], F32) for i in range(4)]
              pO = [ps(pb, "pO%d" % i, [128, 512], F32) for i in range(2)]
              pSS = ps(pb, "pSS", [128, 512], F32)
              for i in range(2):
                  tk.op("pool", lambda v: v.memset(kt[i][64:96, :], 1.0), writes=[("kt", i)])
                  tk.op("pool", lambda v: v.memset(vt[i][:], 0.0), writes=[("vt", i)])
              LA = 3
              items = [(hh, c, j) for hh in range(16) for c in range(NCH) for j in range(4 * c + 4)]

              def load_head(hh):
                  fox = hh < 8
                  h = hh % 8
                  b2 = hh % 2
                  ktb, qtb, vtb = kt[b2], qt[b2], vt[b2]
                  if fox:
                      tk.dma("sp", lambda q: q.dma_start(out=ktb[0:64, :], in_=KF[h, :, :]),
                             reads=[("QKF", 1, h, c) for c in range(NCH)], writes=[("kt", b2)])
                      tk.dma("sp", lambda q: q.dma_start(out=qtb[0:67, :], in_=QF[h, :, :]),
                             reads=[("QKF", 0, h, c) for c in range(NCH)] + [("QFD", c) for c in range(NCH)], writes=[("qt", b2)])
                      tk.dma("sp", lambda q: q.dma_start(out=vtb[:, :, 0:65], in_=VF[h]), reads=[("VF", h, c) for c in range(NCH)], writes=[("vt", b2)])
                  else:
                      tk.dma("sp", lambda q: q.dma_start(out=ktb[0:96, :], in_=KM[h, :, :]),
                             reads=[("KMR", h, c) for c in range(NCH)] + [("KMN", h, c) for c in range(NCH)], writes=[("kt", b2)])
                      tk.dma("sp", lambda q: q.dma_start(out=qtb[0:96, :], in_=QM[h, :, :]), reads=[("QM", h, c) for c in range(NCH)] + [("QMR", h, c) for c in range(NCH)], writes=[("qt", b2)])
                      tk.dma("sp", lambda q: q.dma_start(out=vtb[:, :, 0:65], in_=VM[h]), reads=[("VM", h, c) for c in range(NCH)], writes=[("vt", b2)])

              def emit_qk(n):
                  hh, c, j = items[n]
                  fox = hh < 8
                  R = 67 if fox else 96
                  b2 = hh % 2
                  ktb, qtb = kt[b2], qt[b2]
                  rd = [("kt", b2), ("qt", b2)]
                  q0 = c * 512
                  psb, psn = pS[n % 4], ("pS", n % 4)
                  ptb, ptn = pT[n % 4], ("pT", n % 4)
                  ksl = slice(j * 128, (j + 1) * 128)
                  if j < 4 * c:
                      tk.op("pe", lambda p: p.matmul(psb[:, :], lhsT=ktb[0:R, ksl], rhs=qtb[0:R, q0:q0 + 512], start=True, stop=True),
                            reads=rd, writes=[psn])
                  else:
                      i = j - 4 * c
                      lo = i * 128
                      tk.op("pe", lambda p: p.matmul(psb[:, lo:lo + 128], lhsT=identb[:], rhs=maskb[:], start=True, stop=False),
                            reads=["identb", "maskb"], writes=[psn])
                      tk.op("pe", lambda p: p.matmul(psb[:, lo:lo + 128], lhsT=ktb[0:R, ksl], rhs=qtb[0:R, q0 + lo:q0 + lo + 128],
                                                     start=False, stop=True), reads=rd, writes=[psn])
                      if i < 3:
                          tk.op("pe", lambda p: p.matmul(psb[:, lo + 128:512], lhsT=ktb[0:R, ksl], rhs=qtb[0:R, q0 + lo + 128:q0 + 512],
                                                         start=True, stop=True), reads=rd, writes=[psn])
                      if lo > 0:
                          tk.op("pool", lambda v: v.memset(ptb[:, 0:lo], 0.0), writes=[ptn])

              def emit_exp_pv(n):
                  hh, c, j = items[n]
                  fox = hh < 8
                  h = hh % 8
                  b2 = hh % 2
                  vtb = vt[b2]
                  q0 = c * 512
                  nk = 4 * c + 4
                  psb, psn = pS[n % 4], ("pS", n % 4)
                  ptb, ptn = pT[n % 4], ("pT", n % 4)
                  po, pon = pO[c % 2], ("pO", c % 2)
                  lo = 0 if j < 4 * c else (j - 4 * c) * 128
                  bias_ap = NDp_all[l][:, j, h:h + 1] if fox else 0.0
                  tk.op("act", lambda a: a.activation(out=ptb[:, lo:512], in_=psb[:, lo:512], func=AF.Exp, bias=bias_ap),
                        reads=[psn, "NDp"], writes=[ptn])
                  tk.op("pe", lambda p: p.matmul(po[:, :], lhsT=vtb[:, j, :], rhs=ptb[:, :], start=(j == 0), stop=(j == nk - 1)),
                        reads=[ptn, ("vt", b2)], writes=[pon])
                  if j == nk - 1:
                      kp = kpost[0] % 2
                      kpost[0] += 1
                      nb, nbn = num[kp], ("num", kp)
                      ab, abn = atb[kp], ("atb", kp)
                      sqn, sqnn = sqns[kp], ("sqn", kp)
                      tk.op("dve", lambda v: v.tensor_copy(out=nb[:], in_=po[0:65, :]), reads=[pon], writes=[nbn])
                      tk.op("pool", lambda v: v.tensor_tensor(out=sqn[:], in0=nb[:], in1=nb[:], op=ALU.mult), reads=[nbn], writes=[sqnn])

                      def p2():
                          tk.op("pe", lambda p: p.matmul(pSS[0:64, :], lhsT=wselb[:], rhs=sqn[:], start=True, stop=True), reads=[sqnn, "wselb"], writes=["pSS"])

                      def p3():
                          tk.op("act", lambda a: a.activation(out=lnv[:], in_=pSS[0:64, :], func=AF.Ln), reads=["pSS"], writes=["lnv"])
                          tk.op("act", lambda a: a.activation(out=lnv[:], in_=lnv[:], func=AF.Exp, scale=-0.5), reads=["lnv"], writes=["lnv"])

                      def p4():
                          tk.op("dve", lambda v: v.tensor_tensor(out=ab[:], in0=nb[0:64, :], in1=lnv[:], op=ALU.mult), reads=[nbn, "lnv"], writes=[abn])
                          tk.dma("sp", lambda q: q.dma_start(out=AT[hh, :, q0:q0 + 512], in_=ab[:]), reads=[abn], writes=[("AT", hh, c)])

                      deferred.append((step[0] + 7, p2))
                      deferred.append((step[0] + 10, p3))
                      deferred.append((step[0] + 12, p4))

              load_head(0)
              NI = len(items)
              deferred = []
              step = [0]
              kpost = [0]
              for n in range(NI + LA + 16):
                  step[0] = n
                  due = [d for d in deferred if d[0] <= n]
                  for d in due:
                      deferred.remove(d)
                      d[1]()
                  if n < NI:
                      hh, c, j = items[n]
                      if c == 0 and j == 0 and hh + 1 < 16:
                          pass
                      if c == 1 and j == 0 and hh + 1 < 16:
                          load_head(hh + 1)
                      emit_qk(n)
                  if LA <= n < NI + LA:
                      emit_exp_pv(n - LA)
              assert not deferred
              tk.barrier()

          if stop == "B":
              break
          with ExitStack() as pc:
              gO = sb(pc, "gO", [128, 8], F32)
              gF = sb(pc, "gF", [128, 8], F32)
              wrt = sb(pc, "wrt", [128, 8, 40], F32)
              XNT = sb(pc, "XNT", [128, 8, S], BF16)
              M1 = sb(pc, "M1", [128, NT, 32], F32)
              M2 = sb(pc, "M2", [128, NT, 32], F32)
              G1 = sb(pc, "G1", [128, NT], F32)
              G2 = sb(pc, "G2", [128, NT], F32)
              tk.dma("sp", lambda q: q.dma_start(out=gO[:], in_=gO_in[l]), writes=["gO"])
              tk.dma("sp", lambda q: q.dma_start(out=gF[:], in_=gF_in[l]), writes=["gF"])
              with ExitStack() as pc1:
                  wo_bf = sb(pc1, "wo_bf", [128, 8, D], BF16)
                  wst = sb(pc1, "wst", [128, D], F32)
                  atts = [sb(pc1, "att%d" % i, [128, 8, 512], BF16) for i in range(2)]
                  hxcs = [sb(pc1, "hxC%d" % i, [128, 4, D], F32) for i in range(2)]
                  h1 = [sb(pc1, "h1_%d" % i, [128, D], F32) for i in range(2)]
                  junk = sb(pc1, "junkC", [128, D], F32)
                  xn2s = [sb(pc1, "xn2_%d" % i, [128, D], F32) for i in range(2)]
                  smxs = [sb(pc1, "smx%d" % i, [128, 2], F32) for i in range(2)]
                  xT32s = [sb(pc1, "xT32_%d" % i, [128, 8, 128], F32) for i in range(2)]
                  sm = sb(pc1, "sm", [128, 16], F32)
                  lg = sb(pc1, "lg", [128, 40], F32)
                  r8 = [sb(pc1, "r8_%d" % i, [128, 8], F32) for i in range(3)]
                  r32 = sb(pc1, "r32", [128, 8, 4], F32)
                  r4 = [sb(pc1, "r4_%d" % i, [128, 4], F32) for i in range(5)]
                  py = [ps(pc1, "py%d" % i, [128, 512], F32) for i in range(4)]
                  ptx = [ps(pc1, "ptx%d" % i, [128, 512], F32) for i in range(2)]
                  plg = ps(pc1, "plg", [128, 512], F32)
                  for hh in range(8):
                      tk.dma("sp", lambda q: q.dma_start(out=wst[:], in_=wo_in[l, :, hh, :]), writes=["wst"])
                      tk.op("dve", lambda v: v.tensor_scalar(out=wo_bf[:, hh, :], in0=wst[:], scalar1=gO[:, hh:hh + 1], scalar2=None, op0=ALU.mult),
                            reads=["wst", "gO"], writes=["wo_bf"])
                  tk.dma("sp", lambda q: q.dma_start(out=wrt[:], in_=wrt_in[l]), writes=["wrt"])
                  for k in range(8):
                      tk.op("dve", lambda v: v.tensor_scalar(out=wrt[:, k, :], in0=wrt[:, k, :], scalar1=gF[:, k:k + 1], scalar2=None, op0=ALU.mult),
                            reads=["wrt", "gF"], writes=["wrt"])
                  def c1_load(c):
                      q0 = c * 512
                      att, hx = atts[c % 2], hxcs[c % 2]
                      tk.dma("sp", lambda q: q.dma_start(out=att[:], in_=AT[:, :, q0:q0 + 512].rearrange("(m a) p n -> (a p) m n", a=2)),
                             reads=[("AT", hh, c) for hh in range(16)], writes=[("att", c % 2)])
                      tk.dma("sp", lambda q: q.dma_start(out=hx[:], in_=hsrc[q0:q0 + 512, :].rearrange("(t p) d -> p t d", p=128)),
                             reads=[("hB", l, 4 * c + t_) for t_ in range(4)], writes=[("hxC", c % 2)])

                  def stageX(T):
                      c, t = T // 4, T % 4
                      q0 = c * 512
                      xn2 = xn2s[T % 2]
                      XN2 = ("xn2", T % 2)
                      smx = smxs[T % 2]
                      SMX = ("smx", T % 2)
                      att, hx = atts[c % 2], hxcs[c % 2]
                      ATT, HXC = ("att", c % 2), ("hxC", c % 2)
                      if t == 0 and c + 1 < NCH:
                          c1_load(c + 1)
                      hb, hbn = h1[T % 2], ("h1", T % 2)
                      for half in range(2):
                          pyb, pyn = py[(T % 2) * 2 + half], ("py", (T % 2) * 2 + half)
                          for hh in range(8):
                              tk.op("pe", lambda p: p.matmul(pyb[:, :], lhsT=att[:, hh, t * 128:(t + 1) * 128],
                                                             rhs=wo_bf[:, hh, half * 512:(half + 1) * 512], start=(hh == 0), stop=(hh == 7)),
                                    reads=[ATT, "wo_bf"], writes=[pyn])
                          tk.op("dve", lambda v: v.tensor_tensor(out=hb[:, half * 512:(half + 1) * 512], in0=pyb[:, :],
                                                                 in1=hx[:, t, half * 512:(half + 1) * 512], op=ALU.add),
                                reads=[pyn, HXC], writes=[hbn])
                      tk.dma("sp", lambda q: q.dma_start(out=hcur[T * 128:(T + 1) * 128, :], in_=hb[:]), reads=[hbn], writes=[("hA", T)])
                      tk.op("act", lambda a: a.activation(out=junk[:], in_=hb[:], func=AF.Square), reads=[hbn], writes=["junkC"])
                      tk.op("dve", lambda v: v.tensor_reduce(out=smx[:, 0:1], in_=junk[:], axis=AX.X, op=ALU.add), reads=["junkC"], writes=[SMX])
                      tk.op("act", lambda a: a.activation(out=smx[:, 1:2], in_=smx[:, 0:1], func=AF.Ln, bias=EPS, scale=1.0 / D), reads=[SMX], writes=[SMX])
                      tk.op("act", lambda a: a.activation(out=smx[:, 1:2], in_=smx[:, 1:2], func=AF.Exp, scale=-0.5), reads=[SMX], writes=[SMX])
                      tk.op("dve", lambda v: v.tensor_scalar(out=xn2[:], in0=hb[:], scalar1=smx[:, 1:2], scalar2=None, op0=ALU.mult),
                            reads=[hbn, SMX], writes=[XN2])

                  def stageY(T):
                      c, t = T // 4, T % 4
                      xn2 = xn2s[T % 2]
                      XN2 = ("xn2", T % 2)
                      xT32 = xT32s[T % 2]
                      XT32 = ("xT32", T % 2)
                      for k in range(8):
                          pb_, pbn = ptx[k // 4], ("ptx", k // 4)
                          tk.op("pe", lambda p: p.transpose(out=pb_[:, (k % 4) * 128:(k % 4 + 1) * 128], in_=xn2[:, k * 128:(k + 1) * 128],
                                                           identity=identf), reads=[XN2, "cst"], writes=[pbn])
                          if k % 4 == 3:
                              kk0 = k - 3
                              tk.op("act", lambda a: a.copy(out=xT32[:, kk0:kk0 + 4, :], in_=pb_[:, :].rearrange("p (a n) -> p a n", n=128)),
                                    reads=[pbn], writes=[XT32])
                      tk.op("pool", lambda v: v.tensor_copy(out=XNT[:, :, T * 128:(T + 1) * 128], in_=xT32[:]), reads=[XT32], writes=[("XNT", T)])

                  def stageY2(T):
                      c, t = T // 4, T % 4
                      xT32 = xT32s[T % 2]
                      XT32 = ("xT32", T % 2)
                      for k in range(8):
                          tk.op("pe", lambda p: p.matmul(plg[:, 0:40], lhsT=xT32[:, k, :], rhs=wrt[:, k, :], start=(k == 0), stop=(k == 7)),
                                reads=[XT32, "wrt"], writes=["plg"])
                      V = "dve"
                      tk.op(V, lambda v: v.tensor_copy(out=lg[:], in_=plg[:, 0:40]), reads=["plg"], writes=["rt"])
                      R_ = dict(reads=["rt"], writes=["rt"])
                      tk.op(V, lambda v: v.tensor_reduce(out=sm[:, 2:3], in_=lg[:, 0:8], axis=AX.X, op=ALU.max), **R_)
                      tk.op(V, lambda v: v.tensor_scalar(out=r8[0][:], in0=lg[:, 0:8], scalar1=sm[:, 2:3], scalar2=None, op0=ALU.is_equal), **R_)
                      tk.op(V, lambda v: v.tensor_scalar(out=sm[:, 3:4], in0=sm[:, 2:3], scalar1=-1.0, scalar2=None, op0=ALU.mult), **R_)
                      tk.op("act", lambda a: a.activation(out=r8[1][:], in_=lg[:, 0:8], func=AF.Exp, bias=sm[:, 3:4]), **R_)
                      tk.op(V, lambda v: v.tensor_reduce(out=sm[:, 4:5], in_=r8[1][:], axis=AX.X, op=ALU.add), **R_)
                      tk.op(V, lambda v: v.reciprocal(out=sm[:, 5:6], in_=sm[:, 4:5]), **R_)
                      elv = lg[:, 8:40].rearrange("p (g j) -> p g j", j=4)
                      tk.op(V, lambda v: v.tensor_tensor(out=r32[:], in0=elv, in1=r8[0][:].unsqueeze(2).to_broadcast([128, 8, 4]), op=ALU.mult), **R_)
                      tk.op(V, lambda v: v.tensor_reduce(out=r4[0][:], in_=r32[:].rearrange("p g j -> p j g"), axis=AX.X, op=ALU.add), **R_)
                      tk.op(V, lambda v: v.tensor_reduce(out=sm[:, 6:7], in_=r4[0][:], axis=AX.X, op=ALU.max), **R_)
                      tk.op(V, lambda v: v.tensor_scalar(out=r4[1][:], in0=r4[0][:], scalar1=sm[:, 6:7], scalar2=None, op0=ALU.is_equal), **R_)
                      tk.op(V, lambda v: v.scalar_tensor_tensor(out=r4[2][:], in0=r4[1][:], scalar=-1e30, in1=r4[0][:], op0=ALU.mult, op1=ALU.add), **R_)
                      tk.op(V, lambda v: v.tensor_reduce(out=sm[:, 7:8], in_=r4[2][:], axis=AX.X, op=ALU.max), **R_)
                      tk.op(V, lambda v: v.tensor_scalar(out=r4[3][:], in0=r4[2][:], scalar1=sm[:, 7:8], scalar2=None, op0=ALU.is_equal), **R_)
                      tk.op(V, lambda v: v.tensor_tensor(out=sm[:, 8:9], in0=sm[:, 7:8], in1=sm[:, 6:7], op=ALU.subtract), **R_)
                      tk.op("act", lambda a: a.activation(out=sm[:, 9:10], in_=sm[:, 8:9], func=AF.Exp), **R_)
                      tk.op(V, lambda v: v.tensor_scalar(out=sm[:, 10:11], in0=sm[:, 9:10], scalar1=1.0, scalar2=None, op0=ALU.add), **R_)
                      tk.op(V, lambda v: v.reciprocal(out=sm[:, 11:12], in_=sm[:, 10:11]), **R_)
                      tk.op(V, lambda v: v.tensor_tensor(out=G1[:, T:T + 1], in0=sm[:, 11:12], in1=sm[:, 5:6], op=ALU.mult), reads=["rt"], writes=["rt", "G"])
                      tk.op(V, lambda v: v.scalar_tensor_tensor(out=G2[:, T:T + 1], in0=sm[:, 9:10], scalar=sm[:, 11:12], in1=sm[:, 5:6],
                                                                op0=ALU.mult, op1=ALU.mult), reads=["rt"], writes=["rt", "G"])
                      m1v = M1[:, T, :].rearrange("p (g j) -> p g j", j=4)
                      m2v = M2[:, T, :].rearrange("p (g j) -> p g j", j=4)
                      ohb = r8[0][:].unsqueeze(2).to_broadcast([128, 8, 4])
                      tk.op(V, lambda v: v.tensor_tensor(out=m1v, in0=ohb, in1=r4[1][:].unsqueeze(1).to_broadcast([128, 8, 4]), op=ALU.mult),
                            reads=["rt"], writes=["rt", "M"])
                      tk.op(V, lambda v: v.tensor_tensor(out=m2v, in0=ohb, in1=r4[3][:].unsqueeze(1).to_broadcast([128, 8, 4]), op=ALU.mult),
                            reads=["rt"], writes=["rt", "M"])

                  c1_load(0)
                  stageX(0)
                  for T in range(NT + 1):
                      if T + 1 < NT:
                          stageX(T + 1)
                      if T < NT:
                          stageY(T)
                      if T >= 1:
                          stageY2(T - 1)
                  tk.barrier()

              if stop == "C":
                  break
              with ExitStack() as pd:
                  GT = sb(pd, "GT", [128, NT, 32], F32)
                  wguf = [sb(pd, "wguf%d" % i, [128, 8, 512], F32) for i in range(2)]
                  wdf = [sb(pd, "wdf%d" % i, [128, 2, D], F32) for i in range(2)]
                  wgub = [sb(pd, "wgub%d" % i, [128, 8, 512], BF16) for i in range(2)]
                  wdb = [sb(pd, "wdb%d" % i, [128, 2, D], BF16) for i in range(2)]
                  sg_ = [sb(pd, "sgD%d" % i, [128, 512], F32) for i in range(2)]
                  hT = [sb(pd, "hT%d" % i, [128, 2, 512], BF16) for i in range(2)]
                  yacc = sb(pd, "yacc", [128, 8, D], F32)
                  smf = sb(pd, "smf", [128, 4], F32)
                  if last:
                      hh1 = [sb(pd, "hh1_%d" % i, [128, D], F32) for i in range(1)]
                      junk = sb(pd, "junkF", [128, D], F32)
                      fing = sb(pd, "fing", [128, D], F32)
                  else:
                      tmpy = [sb(pd, "tmpy%d" % i, [128, 512], F32) for i in range(2)]
                  pgu = [ps(pd, "pgu%d" % i, [128, 512], F32) for i in range(4)]
                  pyd = [ps(pd, "pyd%d" % i, [128, 512], F32) for i in range(4)]
                  if last:
                      tk.dma("sp", lambda q: q.dma_start(out=fing[:], in_=fin_in[:, :]), writes=["fing"])
                  for T in range(NT):
                      tk.op("dve", lambda v: v.tensor_scalar(out=GT[:, T, :], in0=M1[:, T, :], scalar1=G1[:, T:T + 1], scalar2=None, op0=ALU.mult),
                            reads=["M", "G"], writes=["GT"])
                      tk.op("dve", lambda v: v.scalar_tensor_tensor(out=GT[:, T, :], in0=M2[:, T, :], scalar=G2[:, T:T + 1], in1=GT[:, T, :],
                                                                    op0=ALU.mult, op1=ALU.add), reads=["M", "G", "GT"], writes=["GT"])

                  def load_w(n):
                      e = n % 32
                      i = n % 2
                      tk.dma("sp", lambda q: q.dma_start(out=wguf[i][:].rearrange("p k n -> p (k n)"), in_=wgu_in[l][e * 128:(e + 1) * 128, :]),
                             writes=[("wguf", i)])
                      tk.dma("sp", lambda q: q.dma_start(out=wdf[i][:].rearrange("p k n -> p (k n)"), in_=wd_in[l][e * 128:(e + 1) * 128, :]),
                             writes=[("wdf", i)])

                  GS = 8
                  NG = NT // GS
                  load_w(0)
                  wcnt = [0]

                  def stage1(grp, e, cc, n, i, fs=range(4)):
                      t0_ = (grp * GS + cc * 4) * 128
                      hb = hT[n % 2]
                      for f in fs:
                          pg, pgn = pgu[f], ("pgu", f)
                          for k in range(8):
                              tk.op("pe", lambda p: p.matmul(pg[:, :], lhsT=wgub[i][:, k, f * 128:(f + 1) * 128], rhs=XNT[:, k, t0_:t0_ + 512],
                                                             start=(k == 0), stop=(k == 7)), reads=[("XNT", grp * GS + cc * 4 + t_) for t_ in range(4)] + [("wgub", i)], writes=[pgn])
                          if f < 2:
                              tk.op("act", lambda a: a.activation(out=sg_[f][:], in_=pg[:, :], func=AF.Silu), reads=[pgn], writes=[("sgD", f)])
                          else:
                              tk.op("dve", lambda v: v.tensor_tensor(out=hb[:, f - 2, :], in0=sg_[f - 2][:], in1=pg[:, :], op=ALU.mult),
                                    reads=[("sgD", f - 2), pgn], writes=[("hT", n % 2)])

                  def stage3(grp, e, cc, n, i, ts=range(4)):
                      hb = hT[n % 2]
                      for t in ts:
                          tt = cc * 4 + t
                          T = grp * GS + tt
                          for half in range(2):
                              q_ = (t * 2 + half) % 4
                              pq_, pqn = pyd[q_], ("pyd", q_)
                              for kk in range(2):
                                  tk.op("pe", lambda p: p.matmul(pq_[:, :], lhsT=hb[:, kk, t * 128:(t + 1) * 128], rhs=wdb[i][:, kk, half * 512:(half + 1) * 512],
                                                                 start=(kk == 0), stop=(kk == 1)), reads=[("hT", n % 2), ("wdb", i)], writes=[pqn])
                              ysl = yacc[:, tt, half * 512:(half + 1) * 512]
                              YR = ("yacc", tt, half)
                              if False and (not last) and half == 1:
                                  if e == 0:
                                      tk.op("act", lambda a: a.activation(out=ysl, in_=pq_[:, :], func=AF.Copy, scale=GT[:, T, e:e + 1]),
                                            reads=[pqn, "GT"], writes=[YR])
                                  else:
                                      tb, tbn = tmpy[t % 2], ("tmpy", t % 2)
                                      tk.op("act", lambda a: a.activation(out=tb[:], in_=pq_[:, :], func=AF.Copy, scale=GT[:, T, e:e + 1]),
                                            reads=[pqn, "GT"], writes=[tbn])
                                      tk.op("pool", lambda v: v.tensor_tensor(out=ysl, in0=ysl, in1=tb[:], op=ALU.add), reads=[tbn, YR], writes=[YR])
                              elif e == 0:
                                  tk.op("dve", lambda v: v.tensor_scalar(out=ysl, in0=pq_[:, :], scalar1=GT[:, T, e:e + 1], scalar2=None, op0=ALU.mult),
                                        reads=[pqn, "GT"], writes=[YR])
                              else:
                                  tk.op("dve", lambda v: v.scalar_tensor_tensor(out=ysl, in0=pq_[:, :], scalar=GT[:, T, e:e + 1], in1=ysl,
                                                                                op0=ALU.mult, op1=ALU.add),
                                        reads=[pqn, YR, "GT"], writes=[YR])

                  def final_norm_group(g_):
                      for tt in range(GS):
                          T = g_ * GS + tt
                          i2 = 0
                          tk.dma("sp", lambda q: q.dma_start(out=hh1[i2][:], in_=hcur[T * 128:(T + 1) * 128, :]), reads=[("hB", l + 1, T)], writes=[("hh1", i2)])
                          tk.op("act", lambda a: a.activation(out=junk[:], in_=hh1[i2][:], func=AF.Square), reads=[("hh1", i2)], writes=["junkF"])
                          tk.op("dve", lambda v: v.tensor_reduce(out=smf[:, 0:1], in_=junk[:], axis=AX.X, op=ALU.add), reads=["junkF"], writes=["smf"])
                          tk.op("act", lambda a: a.activation(out=smf[:, 1:2], in_=smf[:, 0:1], func=AF.Ln, bias=EPS, scale=1.0 / D), reads=["smf"], writes=["smf"])
                          tk.op("act", lambda a: a.activation(out=smf[:, 1:2], in_=smf[:, 1:2], func=AF.Exp, scale=-0.5), reads=["smf"], writes=["smf"])
                          tk.op("dve", lambda v: v.scalar_tensor_tensor(out=hh1[i2][:], in0=hh1[i2][:], scalar=smf[:, 1:2], in1=fing[:],
                                                                        op0=ALU.mult, op1=ALU.mult), reads=[("hh1", i2), "smf", "fing"], writes=[("hh1", i2)])
                          tk.dma("sp", lambda q: q.dma_start(out=y_out[T * 128:(T + 1) * 128, :], in_=hh1[i2][:]), reads=[("hh1", i2)], writes=[("out", T)])

                  NQ = NG * 32

                  def cast_w(q):
                      i = q % 2
                      for k in range(8):
                          tk.op("act", lambda a: a.activation(out=wgub[i][:, k, :], in_=wguf[i][:, k, :], func=AF.Copy, scale=gF[:, k:k + 1]),
                                reads=[("wguf", i), "gF"], writes=[("wgub", i)])
                      tk.op("pool", lambda v: v.tensor_copy(out=wdb[i][:], in_=wdf[i][:]), reads=[("wdf", i)], writes=[("wdb", i)])

                  load_w(1)
                  cast_w(0)
                  for grp in range(NG):
                      its = [(e, cc) for e in range(32) for cc in range(GS // 4)]
                      NI2 = len(its)
                      for n in range(NI2 + 1):
                          for f in range(4):
                              if n < NI2:
                                  e, cc = its[n]
                                  stage1(grp, e, cc, n, (grp * 32 + e) % 2, fs=[f])
                              if n >= 1:
                                  e2, cc2 = its[n - 1]
                                  stage3(grp, e2, cc2, n - 1, (grp * 32 + e2) % 2, ts=[f])
                          if n < NI2 and its[n][1] == 0:
                              q = grp * 32 + its[n][0]
                              if q + 1 < NQ:
                                  cast_w(q + 1)
                              if q + 2 < NQ:
                                  load_w(q + 2)
                      for tt in range(GS):
                          T = grp * GS + tt
                          YRS = [("yacc", tt, 0), ("yacc", tt, 1)]
                          tk.dma("pool", lambda q: q.dma_start(out=hcur[T * 128:(T + 1) * 128, :], in_=yacc[:, tt, :], accum_op=ALU.add),
                                 reads=YRS + [("hA", T)], writes=[("hB", l + 1, T)])
                      if last:
                          if grp >= 1:
                              final_norm_group(grp - 1)
                  if last:
                      final_norm_group(NG - 1)
                  tk.barrier()
          if stop == "L0":
              break
    except _Stop:
        return nc
    es.close()
    return nc


def _host_layout(inp):
    f = np.float32
    g = {k: np.asarray(v) for k, v in inp.items()}
    sh = {}
    sh["gA"] = np.ascontiguousarray(g["attn_norm"].reshape(L, 8, 128).transpose(0, 2, 1)).astype(f)
    sh["gF"] = np.ascontiguousarray(g["ffn_norm"].reshape(L, 8, 128).transpose(0, 2, 1)).astype(f)
    sh["gQ"] = np.ascontiguousarray(g["q_norm"].reshape(L, 2, 128).transpose(0, 2, 1)).astype(f)
    sh["gKV"] = np.ascontiguousarray(g["kv_norm"].reshape(L, 128, 1)).astype(f)
    go = np.concatenate([g["fox_out_norm"], g["mla_out_norm"]], axis=1)
    sh["gO"] = np.ascontiguousarray(go.reshape(L, 8, 128).transpose(0, 2, 1)).astype(f)
    sh["bfb"] = np.ascontiguousarray(np.tile(g["b_f"][:, None, None, :], (1, 128, 4, 1)).reshape(L, 128, 32)).astype(f)
    sh["fing"] = np.ascontiguousarray(np.tile(g["final_norm"][None, :], (128, 1))).astype(f)
    w_in = g["w_in"]
    kr = w_in[:, :, 1928:1960]
    kr_sw = np.concatenate([kr[:, :, 16:32], kr[:, :, 0:16]], axis=2)
    win = np.concatenate([w_in, kr_sw], axis=2)
    sh["win"] = np.ascontiguousarray(win.reshape(L, 8, 128, INW + 32).transpose(0, 2, 1, 3)).astype(f)
    wuq = g["w_uq"]
    rope = wuq.reshape(L, 256, 8, 96)[:, :, :, 64:96]
    rope_sw = np.concatenate([rope[..., 16:32], rope[..., 0:16]], axis=-1).reshape(L, 256, 256)
    wuq2 = np.concatenate([wuq, rope_sw], axis=2)
    sh["wuq"] = np.ascontiguousarray(wuq2.reshape(L, 2, 128, 1024).transpose(0, 2, 1, 3)).astype(f)
    sh["wukv"] = np.ascontiguousarray(g["w_ukv"]).astype(f)
    sh["wo"] = np.ascontiguousarray(g["w_o"].reshape(L, 8, 128, 1024).transpose(0, 2, 1, 3)).astype(f)
    wrt = np.concatenate([g["w_group"], g["w_router"]], axis=2)
    sh["wrt"] = np.ascontiguousarray(wrt.reshape(L, 8, 128, 40).transpose(0, 2, 1, 3)).astype(f)
    for l in range(L):
        gu = np.concatenate([g["w_gate"][l], g["w_up"][l]], axis=2)
        sh["wgu%d" % l] = np.ascontiguousarray(gu.reshape(32, 8, 128, 512).transpose(0, 2, 1, 3).reshape(4096, 4096)).astype(f)
        wd = g["w_down"][l]
        sh["wd%d" % l] = np.ascontiguousarray(wd.reshape(32, 2, 128, 1024).transpose(0, 2, 1, 3).reshape(4096, 2048)).astype(f)
    cst = np.zeros((128, 6, 128), f)
    cst[:, 0, :] = np.eye(128, dtype=f)
    ii = np.arange(128)
    cst[:, 1, :] = (ii[:, None] <= ii[None, :]).astype(f)
    cst[:, 2, :] = (ii[:, None] < ii[None, :]).astype(f)
    cst[:, 3, :] = 1.0
    cst[:, 4, :] = np.where(ii[:, None] > ii[None, :], -30000.0, 0.0).astype(f)
    cst[0:64, 5, 0:64] = 1.0 / 64.0
    cst[64, 5, 0:64] = EPS
    sh["cst"] = cst
    cst2 = np.zeros((128, 4 + NBLK), f)
    half = 16
    inv_freq = (10000.0 ** (-np.arange(half, dtype=np.float64) / half))
    cst2[:, 0] = (inv_freq[ii % 16] / (2.0 * np.pi)).astype(f)
    cst2[:, 1] = ii.astype(f)
    cst2[:, 4:] = (np.arange(NBLK) * 128).astype(f)[None, :]
    sh["cst2"] = cst2
    return sh


_CACHE = {}


def kernel(**inputs):
    x = np.asarray(inputs["x"], dtype=np.float32)
    pos = np.asarray(inputs["positions"]).astype(np.int32)
    shared = _host_layout(inputs)
    if "nc" not in _CACHE:
        _CACHE["nc"] = build_program()
    nc = _CACHE["nc"]
    in_maps = []
    for b in range(8):
        m = dict(shared)
        m["x"] = np.ascontiguousarray(x[b])
        m["pos"] = np.ascontiguousarray(np.tile(pos[b][None, :], (128, 1)))
        in_maps.append(m)
    res = run_bass_kernel_spmd(nc, in_maps, core_ids=list(range(8)))
    out = np.stack([np.asarray(r["y"]) for r in res.results], axis=0)
    return out.astype(np.float32)
```

```python
import math
from contextlib import ExitStack

import numpy as np
import ml_dtypes
import concourse.bass as bass
import concourse.mybir as mybir
from concourse.bass_utils import run_bass_kernel_spmd

F32, BF16, I32 = mybir.dt.float32, mybir.dt.bfloat16, mybir.dt.int32
AF = mybir.ActivationFunctionType
ALU = mybir.AluOpType
AX = mybir.AxisListType

S = 4096
D = 1024
L = 2
NT = S // 128
NCH = S // 512
INW = 1960
NBLK = 96
NROWS = NBLK * 128
EPS = 1e-6
TWO_PI = 2.0 * math.pi


class TK:
    def __init__(self, nc, es):
        self.nc = nc
        self.E = dict(pe=nc.tensor, act=nc.scalar, dve=nc.vector, pool=nc.gpsimd, sp=nc.sync)
        self.sem = {k: es.enter_context(nc.semaphore("sem_" + k)) for k in self.E}
        self.cnt = {k: 0 for k in self.E}
        self.seen = {k: {} for k in self.E}
        self.lastw = {}
        self.readers = {}
        self.NS = 40
        self.dsem = [es.enter_context(nc.semaphore("dsem%d" % i)) for i in range(self.NS)]
        self.dma_i = 0

    def _semof(self, src):
        return self.dsem[src[1]] if isinstance(src, tuple) else self.sem[src]

    def _wait(self, e, tok):
        src, val = tok
        if src == "pe" and e == "pe":
            return
        if self.seen[e].get(src, 0) >= val:
            return
        self.E[e].wait_ge(self._semof(src), val)
        self.seen[e][src] = val

    def _deps(self, e, reads, writes):
        for r in reads:
            t = self.lastw.get(r)
            if t:
                self._wait(e, t)
        for w in writes:
            t = self.lastw.get(w)
            if t:
                self._wait(e, t)
            for src, val in list(self.readers.get(w, {}).items()):
                self._wait(e, (src, val))

    def _commit(self, tok, reads, writes):
        for r in reads:
            d = self.readers.setdefault(r, {})
            d[tok[0]] = max(d.get(tok[0], 0), tok[1])
        for w in writes:
            self.lastw[w] = tok
            self.readers[w] = {}

    PSUM_T = {"ptrA", "paccA", "pS", "pO", "py", "ptx", "pgu", "pht", "pyd"}
    PSUM_S = {"pfl", "pdt", "pSS", "plg"}

    def _is_psum(self, r):
        return (isinstance(r, tuple) and r[0] in self.PSUM_T) or (isinstance(r, str) and r in self.PSUM_S)

    def op(self, e, fn, reads=(), writes=()):
        px = [r for r in reads if self._is_psum(r)]
        if px:
            reads = [r for r in reads if not self._is_psum(r)]
            writes = list(writes) + [r for r in px if r not in writes]
        self._deps(e, reads, writes)
        ins = fn(self.E[e])
        self.cnt[e] += 1
        ins.then_inc(self.sem[e], 1)
        tok = (e, self.cnt[e])
        self._commit(tok, reads, writes)
        return tok

    def dma(self, e, fn, reads=(), writes=()):
        self._deps(e, reads, writes)
        i = self.dma_i
        self.dma_i += 1
        slot, k = i % self.NS, i // self.NS
        src = ("d", slot)
        if k > 0:
            self._wait(e, (src, 16 * k))
        ins = fn(self.E[e])
        ins.then_inc(self.dsem[slot], 16)
        tok = (src, 16 * (k + 1))
        self._commit(tok, reads, writes)
        return tok

    def barrier(self):
        for e in ("pe", "act", "dve", "pool", "sp"):
            for t in list(self.lastw.values()):
                self._wait(e, t)
            for d in list(self.readers.values()):
                for src, val in list(d.items()):
                    self._wait(e, (src, val))

    def wait_all(self, e, resources):
        for r in resources:
            t = self.lastw.get(r)
            if t:
                self._wait(e, t)


def build_program(debug=False, stop=None):
    nc = bass.Bass("TRN2", target_bir_lowering=False)
    try:
        nc.allow_low_precision("bf16 matmul operands with fp32 accumulation")
    except Exception:
        pass
    try:
        nc.allow_non_contiguous_dma("small strided layout DMAs")
    except Exception:
        pass

    def din(name, shape, dt=F32):
        return nc.dram_tensor(name, list(shape), dt, kind="ExternalInput").ap()

    def dscr(name, shape, dt):
        return nc.dram_tensor(name, list(shape), dt, kind="ExternalOutput" if debug else "Internal").ap()

    x_in = din("x", [S, D])
    pos_in = din("pos", [128, S], I32)
    gA_in = din("gA", [L, 128, 8])
    gF_in = din("gF", [L, 128, 8])
    gQ_in = din("gQ", [L, 128, 2])
    gKV_in = din("gKV", [L, 128, 1])
    gO_in = din("gO", [L, 128, 8])
    bf_in = din("bfb", [L, 128, 32])
    fin_in = din("fing", [128, D])
    win_in = din("win", [L, 128, 8, INW + 32])
    wuq_in = din("wuq", [L, 128, 2, 768 + 256])
    wukv_in = din("wukv", [L, 128, 1024])
    wo_in = din("wo", [L, 128, 8, 1024])
    wrt_in = din("wrt", [L, 128, 8, 40])
    wgu_in = [din("wgu%d" % l, [4096, 8 * 512]) for l in range(L)]
    wd_in = [din("wd%d" % l, [4096, 2 * 1024]) for l in range(L)]
    cst_in = din("cst", [128, 6, 128])
    cst2_in = din("cst2", [128, 4 + NBLK])
    y_out = nc.dram_tensor("y", [S, D], F32, kind="ExternalOutput").ap()

    hA = dscr("hA", [S, D], F32)
    hB = dscr("hB", [S, D], F32)
    QF = dscr("QF", [8, 67, S], BF16)
    KF = dscr("KF", [8, 64, S], BF16)
    VF = dscr("VF", [8, 128, NT, 65], BF16)
    QM = dscr("QM", [8, 96, S], BF16)
    KM = dscr("KM", [8, 96, S], BF16)
    VM = dscr("VM", [8, 128, NT, 65], BF16)
    AT = dscr("AT", [16, 64, S], BF16)
    XB = dscr("XB", [NROWS, D], BF16)
    YB = dscr("YB", [NROWS, D], F32)

    es = ExitStack()
    tk = TK(nc, es)

    uid = [0]

    def sb(es_, name, shape, dt):
        uid[0] += 1
        return es_.enter_context(nc.sbuf_tensor("s%d_%s" % (uid[0], name), list(shape), dt))

    def ps(es_, name, shape, dt=F32):
        uid[0] += 1
        return es_.enter_context(nc.psum_tensor("p%d_%s" % (uid[0], name), list(shape), dt))

    cst = sb(es, "cst", [128, 6, 128], F32)
    cst2 = sb(es, "cst2", [128, 4 + NBLK], F32)
    identb = sb(es, "identb", [128, 128], BF16)
    maskb = sb(es, "maskb", [128, 128], BF16)
    NDp_all = [sb(es, "NDp%d" % i, [128, NT, 8], F32) for i in range(L)]
    tk.dma("sp", lambda q: q.dma_start(out=cst[:], in_=cst_in[:, :, :]), writes=["cst"])
    tk.dma("sp", lambda q: q.dma_start(out=cst2[:], in_=cst2_in[:, :]), writes=["cst2"])
    identf = cst[:, 0, :]
    Uincl = cst[:, 1, :]
    Ustrict = cst[:, 2, :]
    onesf = cst[:, 3, :]
    wselb = sb(es, "wselb", [65, 64], BF16)
    tk.op("dve", lambda v: v.tensor_copy(out=identb[:], in_=cst[:, 0, :]), reads=["cst"], writes=["identb"])
    tk.op("dve", lambda v: v.tensor_copy(out=maskb[:], in_=cst[:, 4, :]), reads=["cst"], writes=["maskb"])
    tk.op("dve", lambda v: v.tensor_copy(out=wselb[:], in_=cst[0:65, 5, 0:64]), reads=["cst"], writes=["wselb"])

    class _Stop(Exception):
        pass

    def chk(name):
        if stop == name:
            tk.barrier()
            raise _Stop()

    try:
      for l in range(L):
          hsrc = x_in if l == 0 else (hA if l % 2 == 1 else hB)
          hcur = hA if l % 2 == 0 else hB
          last = (l == L - 1)
          with ExitStack() as pa:
              win_bf = sb(pa, "win_bf", [128, 8, INW + 32], BF16)
              wqn_bf = sb(pa, "wqn_bf", [128, 2, 512], BF16)
              wqr_bf = sb(pa, "wqr_bf", [128, 2, 256], BF16)
              wqt_bf = sb(pa, "wqt_bf", [128, 2, 256], BF16)
              wukv_bf = sb(pa, "wukv_bf", [128, 1024], BF16)
              wkn_bf = sb(pa, "wkn_bf", [128, 512], BF16)
              gA = sb(pa, "gA", [128, 8], F32)
              posi = sb(pa, "posi", [128, 512], I32)
              gQ = sb(pa, "gQ", [128, 2], F32)
              gKV = sb(pa, "gKV", [128, 1], F32)
              bfb = sb(pa, "bfb", [128, 32], F32)
              wstg = [sb(pa, "wstg%d" % i, [128, INW + 32], F32) for i in range(2)]
              tk.dma("sp", lambda q: q.dma_start(out=gA[:], in_=gA_in[l]), writes=["gA"])
              tk.dma("sp", lambda q: q.dma_start(out=gQ[:], in_=gQ_in[l]), writes=["gQ"])
              tk.dma("sp", lambda q: q.dma_start(out=gKV[:], in_=gKV_in[l]), writes=["gKV"])
              tk.dma("sp", lambda q: q.dma_start(out=bfb[:], in_=bf_in[l]), writes=["bfb"])
              for k in range(8):
                  st = wstg[k % 2]
                  rs = ("wstg", k % 2)
                  tk.dma("sp", lambda q: q.dma_start(out=st[:], in_=win_in[l, :, k, :]), writes=[rs])
                  tk.op("dve", lambda v: v.tensor_scalar(out=win_bf[:, k, 0:512], in0=st[:, 0:512], scalar1=gA[:, k:k + 1],
                                                         scalar2=0.125, op0=ALU.mult, op1=ALU.mult),
                        reads=[rs, "gA"], writes=["win_bf"])
                  tk.op("dve", lambda v: v.tensor_scalar(out=win_bf[:, k, 512:INW], in0=st[:, 512:INW],
                                                         scalar1=gA[:, k:k + 1], scalar2=None, op0=ALU.mult),
                        reads=[rs, "gA"], writes=["win_bf"])
                  tk.op("dve", lambda v: v.tensor_scalar(out=win_bf[:, k, INW:INW + 16], in0=st[:, INW:INW + 16],
                                                         scalar1=gA[:, k:k + 1], scalar2=-1.0, op0=ALU.mult, op1=ALU.mult),
                        reads=[rs, "gA"], writes=["win_bf"])
                  tk.op("dve", lambda v: v.tensor_scalar(out=win_bf[:, k, INW + 16:INW + 32], in0=st[:, INW + 16:INW + 32],
                                                         scalar1=gA[:, k:k + 1], scalar2=None, op0=ALU.mult),
                        reads=[rs, "gA"], writes=["win_bf"])
              qs = 96.0 ** -0.5
              for kk in range(2):
                  st = wstg[kk % 2]
                  rs = ("wstg", kk % 2)
                  tk.dma("sp", lambda q: q.dma_start(out=st[:, 0:1024], in_=wuq_in[l, :, kk, :]), writes=[rs])
                  sv = st[:, 0:768].rearrange("p (h c) -> p h c", c=96)
                  tk.op("dve", lambda v: v.tensor_scalar(out=wqn_bf[:, kk, :].rearrange("p (h c) -> p h c", c=64), in0=sv[:, :, 0:64],
                                                         scalar1=gQ[:, kk:kk + 1], scalar2=qs, op0=ALU.mult, op1=ALU.mult),
                        reads=[rs, "gQ"], writes=["wuq_bf"])
                  tk.op("dve", lambda v: v.tensor_scalar(out=wqr_bf[:, kk, :].rearrange("p (h c) -> p h c", c=32), in0=sv[:, :, 64:96],
                                                         scalar1=gQ[:, kk:kk + 1], scalar2=qs, op0=ALU.mult, op1=ALU.mult),
                        reads=[rs, "gQ"], writes=["wuq_bf"])
                  rv = st[:, 768:1024].rearrange("p (h c) -> p h c", c=32)
                  wt = wqt_bf[:, kk, :].rearrange("p (h c) -> p h c", c=32)
                  tk.op("dve", lambda v: v.tensor_scalar(out=wt[:, :, 0:16], in0=rv[:, :, 0:16], scalar1=gQ[:, kk:kk + 1],
                                                         scalar2=-qs, op0=ALU.mult, op1=ALU.mult),
                        reads=[rs, "gQ"], writes=["wuqr_bf"])
                  tk.op("dve", lambda v: v.tensor_scalar(out=wt[:, :, 16:32], in0=rv[:, :, 16:32], scalar1=gQ[:, kk:kk + 1],
                                                         scalar2=qs, op0=ALU.mult, op1=ALU.mult),
                        reads=[rs, "gQ", "wuqr_bf"], writes=["wuqr_bf"])
              st = wstg[0]
              tk.dma("sp", lambda q: q.dma_start(out=st[:, 0:1024], in_=wukv_in[l]), writes=[("wstg", 0)])
              tk.op("dve", lambda v: v.tensor_scalar(out=wkn_bf[:].rearrange("p (h c) -> p h c", c=64),
                                                     in0=st[:, 0:1024].rearrange("p (h c) -> p h c", c=128)[:, :, 0:64],
                                                     scalar1=gKV[:, 0:1], scalar2=None, op0=ALU.mult), reads=[("wstg", 0), "gKV"], writes=["wukv_bf"])
              tk.op("dve", lambda v: v.tensor_scalar(out=wukv_bf[:], in0=st[:, 0:1024], scalar1=gKV[:, 0:1], scalar2=None,
                                                     op0=ALU.mult), reads=[("wstg", 0), "gKV"], writes=["wukv_bf"])
              W_ALL = ["win_bf", "wuq_bf", "wuqr_bf", "wukv_bf"]
              chk("A0")

              NDtok = NDp_all[l]
              hxs = [sb(pa, "hx%d" % i, [128, 4, D], F32) for i in range(2)]
              junk = sb(pa, "junk", [128, D], F32)
              ss = sb(pa, "ss", [128, 4], F32)
              rstd = sb(pa, "rstd", [128, 4], F32)
              xns = [sb(pa, "xn%d" % i, [128, 4, D], BF16) for i in range(2)]
              xnTs = [sb(pa, "xnT%d" % i, [128, 8, 512], BF16) for i in range(2)]
              stg = [sb(pa, "stg%d" % i, [128, 512], BF16) for i in range(4)]
              vstf = sb(pa, "vstf", [128, 4, 8, 65], BF16)
              vstm = sb(pa, "vstm", [128, 4, 8, 65], BF16)
              zf = sb(pa, "zf", [128, 32], F32)
              nl = sb(pa, "nl", [128, 32], F32)
              SX = [sb(pa, "SX%d" % i, [128, 5, 8], F32) for i in range(2)]
              dq = sb(pa, "dq", [8, 3, 512], BF16)
              dr = [sb(pa, "dr%d" % i, [8, 512], F32) for i in range(2)]
              cq32 = sb(pa, "cq32", [128, 3, 512], F32)
              sq32 = sb(pa, "sq32", [128, 3, 512], F32)
              rsq = sb(pa, "rsq", [128, 2, 512], F32)
              cqn = sb(pa, "cqn", [128, 3, 512], BF16)
              posf = sb(pa, "posf", [128, 512], F32)
              rr = sb(pa, "rr", [128, 2, 512], F32)
              ri = sb(pa, "ri", [128, 2, 512], I32)
              rf = sb(pa, "rf", [128, 2, 512], F32)
              cs = sb(pa, "cs", [128, 2, 512], F32)
              t1 = sb(pa, "t1", [128, 512], F32)
              t2 = sb(pa, "t2", [128, 512], F32)
              kpe = sb(pa, "kpe", [32, 512], BF16)
              ptr = [ps(pa, "ptrA%d" % i, [128, 512], BF16) for i in range(2)]
              pacc = [ps(pa, "paccA%d" % i, [128, 512], F32) for i in range(4)]
              pfl = ps(pa, "pflA", [128, 512], F32)
              pdt = ps(pa, "pdtA", [128, 512], F32)

              tk.op("pool", lambda v: v.memset(vstf[:], 1.0), writes=["vstf"])
              tk.op("pool", lambda v: v.memset(vstm[:], 1.0), writes=["vstm"])
              tk.op("pool", lambda v: v.memset(SX[0][:], 0.0), writes=[("SX", 0)])

              acc_i = [0]

              def next_acc():
                  i = acc_i[0] % 4
                  acc_i[0] += 1
                  return pacc[i], ("paccA", i)

              stg_i = [0]

              def next_stg():
                  i = stg_i[0] % 4
                  stg_i[0] += 1
                  return stg[i], ("stg", i)

              ev_i = [0]

              def evac(out_ap, in_ap, reads, writes):
                  ev_i[0] += 1
                  if ev_i[0] % 2 == 0:
                      return tk.op("act", lambda a: a.copy(out=out_ap, in_=in_ap), reads=reads, writes=writes)
                  return tk.op("dve", lambda v: v.tensor_copy(out=out_ap, in_=in_ap), reads=reads, writes=writes)

              def pro_load(c):
                  hx = hxs[c % 2]
                  HX = ("hx", c % 2)
                  cs0 = c * 512
                  tk.dma("sp", lambda q: q.dma_start(out=hx[:], in_=hsrc[cs0:cs0 + 512, :].rearrange("(t p) d -> p t d", p=128)),
                         reads=[("hB", l, 4 * c + t_) for t_ in range(4)], writes=[HX])

              def prologue(c, parts=range(9)):
                  hx, xn, xnT = hxs[c % 2], xns[c % 2], xnTs[c % 2]
                  HX, XNr, XNT_ = ("hx", c % 2), ("xn", c % 2), ("xnT", c % 2)
                  for part in parts:
                      if part < 4:
                          t = part
                          tk.op("act", lambda a: a.activation(out=junk[:], in_=hx[:, t, :], func=AF.Square), reads=[HX], writes=["junk"])
                          tk.op("dve", lambda v: v.tensor_reduce(out=ss[:, t:t + 1], in_=junk[:], axis=AX.X, op=ALU.add),
                                reads=["junk"], writes=["ss"])
                      elif part == 4:
                          tk.op("act", lambda a: a.activation(out=rstd[:], in_=ss[:], func=AF.Ln, bias=EPS, scale=1.0 / D),
                                reads=["ss"], writes=["rstd"])
                          tk.op("act", lambda a: a.activation(out=rstd[:], in_=rstd[:], func=AF.Exp, scale=-0.5), reads=["rstd"], writes=["rstd"])
                      else:
                          t = part - 5
                          tk.op("dve", lambda v: v.tensor_scalar(out=xn[:, t, :], in0=hx[:, t, :], scalar1=rstd[:, t:t + 1], scalar2=None,
                                                                 op0=ALU.mult), reads=[HX, "rstd"], writes=[XNr])

              def prologue2(c):
                  xn, xnT = xns[c % 2], xnTs[c % 2]
                  XNr, XNT_ = ("xn", c % 2), ("xnT", c % 2)
                  for k in range(8):
                      pt, ptn = ptr[k % 2], ("ptrA", k % 2)
                      for t in range(4):
                          tk.op("pe", lambda p: p.transpose(out=pt[:, t * 128:(t + 1) * 128], in_=xn[:, t, k * 128:(k + 1) * 128],
                                                           identity=identb[:]), reads=[XNr, "identb"], writes=[ptn])
                      evac(xnT[:, k, :], pt[:, :], [ptn], [XNT_])


              def body(c):
                  cs0 = c * 512
                  csl = slice(cs0, cs0 + 512)
                  xnT = xnTs[c % 2]
                  XNT_ = ("xnT", c % 2)
                  chk("A2")
                  for t in range(4):
                      acc, accn = next_acc()
                      for k in range(8):
                          tk.op("pe", lambda p: p.matmul(acc[:, :], lhsT=xnT[:, k, t * 128:(t + 1) * 128], rhs=win_bf[:, k, 1024:1536],
                                                         start=(k == 0), stop=(k == 7)), reads=[XNT_] + W_ALL, writes=[accn])
                      evac(vstf[:, t, :, 0:64], acc[:, :].rearrange("p (h e) -> p h e", e=64), [accn], ["vstf"])
                      for k in range(8):
                          tk.op("pe", lambda p: p.matmul(pfl[:, t * 8:(t + 1) * 8], lhsT=xnT[:, k, t * 128:(t + 1) * 128],
                                                         rhs=win_bf[:, k, 1536:1544], start=(k == 0), stop=(k == 7)),
                                reads=[XNT_] + W_ALL, writes=["pfl"])
                  for h in range(8):
                      tk.dma("pool", lambda q: q.dma_start(out=VF[h, :, 4 * c:4 * c + 4, :], in_=vstf[:, :, h, :]),
                             reads=["vstf"], writes=[("VF", h, c)])
                  chk("A3")
                  tk.op("dve", lambda v: v.tensor_tensor(out=zf[:], in0=pfl[:, 0:32], in1=bfb[:], op=ALU.add),
                        reads=["pfl", "bfb"], writes=["zf"])
                  tk.op("act", lambda a: a.activation(out=zf[:], in_=zf[:], func=AF.Exp, scale=-1.0), reads=["zf"], writes=["zf"])
                  tk.op("act", lambda a: a.activation(out=nl[:], in_=zf[:], func=AF.Ln, bias=1.0), reads=["zf"], writes=["nl"])
                  sxc, sxn = SX[c % 2], SX[(c + 1) % 2]
                  for t in range(4):
                      tk.op("dve", lambda v: v.tensor_tensor(out=sxc[:, t + 1, :], in0=sxc[:, t, :], in1=nl[:, t * 8:(t + 1) * 8], op=ALU.add),
                            reads=["nl", ("SX", c % 2)], writes=[("SX", c % 2)])
                  tk.op("dve", lambda v: v.tensor_copy(out=sxn[:, 0, :], in_=sxc[:, 4, :]), reads=[("SX", c % 2)], writes=[("SX", (c + 1) % 2)])
                  chk("A4")
                  for m in range(3):
                      acc, accn = next_acc()
                      col0 = 1544 + m * 128
                      for k in range(8):
                          tk.op("pe", lambda p: p.matmul(acc[:, :], lhsT=win_bf[:, k, col0:col0 + 128], rhs=xnT[:, k, :],
                                                         start=(k == 0), stop=(k == 7)), reads=[XNT_] + W_ALL, writes=[accn])
                      tk.op("act", lambda a: a.activation(out=sq32[:, m, :], in_=acc[:, :], func=AF.Square), reads=[accn], writes=["sq32"])
                      tk.op("dve", lambda v: v.tensor_copy(out=cq32[:, m, :], in_=acc[:, :]), reads=[accn, "sq32"], writes=["cq32"])
                  chk("A4a")
                  acc, accn = next_acc()
                  tk.op("pe", lambda p: p.matmul(acc[:, :], lhsT=onesf, rhs=sq32[:, 0, :], start=True, stop=False), reads=["sq32", "cst"], writes=[accn])
                  tk.op("pe", lambda p: p.matmul(acc[:, :], lhsT=onesf, rhs=sq32[:, 1, :], start=False, stop=True), reads=["sq32", "cst"], writes=[accn])
                  tk.op("act", lambda a: a.activation(out=rsq[:, 0, :], in_=acc[:, :], func=AF.Ln, bias=EPS, scale=1.0 / 256), reads=[accn], writes=["rsq"])
                  acc, accn = next_acc()
                  tk.op("pe", lambda p: p.matmul(acc[:, :], lhsT=onesf, rhs=sq32[:, 2, :], start=True, stop=True), reads=["sq32", "cst"], writes=[accn])
                  tk.op("act", lambda a: a.activation(out=rsq[:, 1, :], in_=acc[:, :], func=AF.Ln, bias=EPS, scale=1.0 / 128), reads=[accn], writes=["rsq"])
                  chk("A4b")
                  tk.op("act", lambda a: a.activation(out=rsq[:], in_=rsq[:], func=AF.Exp, scale=-0.5), reads=["rsq"], writes=["rsq"])
                  chk("A4c")
                  for m in range(3):
                      tk.op("dve", lambda v: v.tensor_tensor(out=cqn[:, m, :], in0=cq32[:, m, :], in1=rsq[:, 0 if m < 2 else 1, :], op=ALU.mult),
                            reads=["cq32", "rsq"], writes=["cqn"])
                  chk("A5")
                  tk.dma("sp", lambda q: q.dma_start(out=posi[:], in_=pos_in[:, csl]), writes=["posi"])
                  tk.op("dve", lambda v: v.tensor_copy(out=posf[:], in_=posi[:]), reads=["posi"], writes=["posf"])
                  tk.op("dve", lambda v: v.tensor_scalar(out=rr[:, 0, :], in0=posf[:], scalar1=cst2[:, 0:1], scalar2=None, op0=ALU.mult),
                        reads=["posf", "cst2"], writes=["rr"])
                  tk.op("dve", lambda v: v.tensor_scalar(out=rr[:, 1, :], in0=rr[:, 0, :], scalar1=0.25, scalar2=None, op0=ALU.add),
                        reads=["rr"], writes=["rr"])
                  tk.op("dve", lambda v: v.tensor_copy(out=ri[:], in_=rr[:]), reads=["rr"], writes=["ri"])
                  tk.op("dve", lambda v: v.tensor_copy(out=rf[:], in_=ri[:]), reads=["ri"], writes=["rf"])
                  tk.op("dve", lambda v: v.tensor_tensor(out=rr[:], in0=rr[:], in1=rf[:], op=ALU.subtract), reads=["rr", "rf"], writes=["rr"])
                  tk.op("act", lambda a: a.activation(out=cs[:], in_=rr[:], func=AF.Sin, scale=TWO_PI * (1.0 - 2e-6)), reads=["rr"], writes=["cs"])
                  chk("A6")
                  acc, accn = next_acc()
                  acc2, accn2 = next_acc()
                  for k in range(8):
                      tk.op("pe", lambda p: p.matmul(acc[0:32, :], lhsT=win_bf[:, k, 1928:1960], rhs=xnT[:, k, :], start=(k == 0), stop=(k == 7)),
                            reads=[XNT_] + W_ALL, writes=[accn])
                  for k in range(8):
                      tk.op("pe", lambda p: p.matmul(acc2[0:32, :], lhsT=win_bf[:, k, INW:INW + 32], rhs=xnT[:, k, :], start=(k == 0), stop=(k == 7)),
                            reads=[XNT_] + W_ALL, writes=[accn2])
                  tk.op("dve", lambda v: v.tensor_tensor(out=t1[0:32, :], in0=acc[0:32, :], in1=cs[0:32, 1, :], op=ALU.mult), reads=[accn, "cs"], writes=["t1"])
                  tk.op("dve", lambda v: v.tensor_tensor(out=t2[0:32, :], in0=acc2[0:32, :], in1=cs[0:32, 0, :], op=ALU.mult), reads=[accn2, "cs"], writes=["t2"])
                  tk.op("dve", lambda v: v.tensor_tensor(out=kpe[:], in0=t1[0:32, :], in1=t2[0:32, :], op=ALU.add), reads=["t1", "t2"], writes=["kpe"])
                  for h in range(8):
                      tk.dma("pool", lambda q: q.dma_start(out=KM[h, 64:96, csl], in_=kpe[:]), reads=["kpe"], writes=[("KMR", h, c)])
                  sxc = SX[c % 2]
                  SXN = ("SX", c % 2)
                  for t in range(4):
                      nlt = nl[:, t * 8:(t + 1) * 8]
                      tk.op("pe", lambda p: p.matmul(pfl[:, 64 + t * 8:72 + t * 8],
                                                     lhsT=Uincl, rhs=nlt, start=True, stop=False), reads=["nl", "cst"], writes=["pfl"])
                      tk.op("pe", lambda p: p.matmul(pfl[:, 64 + t * 8:72 + t * 8], lhsT=onesf, rhs=sxc[:, t, :], start=False, stop=True),
                            reads=[SXN, "cst"], writes=["pfl"])
                      tk.op("pe", lambda p: p.matmul(pdt[0:8, t * 128:(t + 1) * 128], lhsT=nlt, rhs=Uincl, start=True, stop=False),
                            reads=["nl", "cst"], writes=["pdt"])
                      tk.op("pe", lambda p: p.matmul(pdt[0:8, t * 128:(t + 1) * 128], lhsT=sxc[:, t, :], rhs=onesf, start=False, stop=True),
                            reads=[SXN, "cst"], writes=["pdt"])
                  tk.op("dve", lambda v: v.tensor_copy(out=NDtok[:, 4 * c:4 * c + 4, :], in_=pfl[:, 64:96].rearrange("p (t h) -> p t h", h=8)),
                        reads=["pfl"], writes=["NDp"])
                  tk.op("dve", lambda v: v.tensor_scalar(out=dq[:, 0, :], in0=pdt[0:8, :], scalar1=-1.0, scalar2=None, op0=ALU.mult),
                        reads=["pdt"], writes=["dq"])
                  tk.op("dve", lambda v: v.scalar_tensor_tensor(out=dr[0][:], in0=pdt[0:8, :], scalar=-1.0, in1=dq[:, 0, :],
                                                                op0=ALU.mult, op1=ALU.subtract), reads=["pdt", "dq"], writes=["dr0"])
                  tk.op("dve", lambda v: v.tensor_copy(out=dq[:, 1, :], in_=dr[0][:]), reads=["dr0"], writes=["dq"])
                  tk.op("dve", lambda v: v.tensor_tensor(out=dr[1][:], in0=dr[0][:], in1=dq[:, 1, :], op=ALU.subtract),
                        reads=["dr0", "dq"], writes=["dr1"])
                  tk.op("dve", lambda v: v.tensor_copy(out=dq[:, 2, :], in_=dr[1][:]), reads=["dr1"], writes=["dq"])
                  tk.dma("pool", lambda q: q.dma_start(out=QF[:, 64:67, csl], in_=dq[:]), reads=["dq"], writes=[("QFD", c)])

                  chk("A1")
                  for kind in range(2):
                      for m in range(4):
                          acc, accn = next_acc()
                          col0 = kind * 512 + m * 128
                          for k in range(8):
                              tk.op("pe", lambda p: p.matmul(acc[:, :], lhsT=win_bf[:, k, col0:col0 + 128], rhs=xnT[:, k, :],
                                                             start=(k == 0), stop=(k == 7)), reads=[XNT_] + W_ALL, writes=[accn])
                          sg, sgn = next_stg()
                          evac(sg[:], acc[:, :], [accn], [sgn])
                          dst = QF if kind == 0 else KF
                          for hh in range(2):
                              h = 2 * m + hh
                              tk.dma("sp", lambda q: q.dma_start(out=dst[h, 0:64, csl], in_=sg[hh * 64:(hh + 1) * 64, :]),
                                     reads=[sgn], writes=[("QKF", kind, h, c)])
                          if c + 1 < NCH:
                              g_ = kind * 4 + m
                              prologue(c + 1, parts={0: [0, 1], 1: [2, 3, 4], 2: [5], 3: [6], 4: [7], 5: [8]}.get(g_, []))
                  chk("A7")
                  for m in range(4):
                      acc, accn = next_acc()
                      for kk in range(2):
                          tk.op("pe", lambda p: p.matmul(acc[:, :], lhsT=wqn_bf[:, kk, m * 128:(m + 1) * 128], rhs=cqn[:, kk, :],
                                                         start=(kk == 0), stop=(kk == 1)), reads=["cqn"] + W_ALL, writes=[accn])
                      sg, sgn = next_stg()
                      evac(sg[:], acc[:, :], [accn], [sgn])
                      for hh in range(2):
                          h = 2 * m + hh
                          tk.dma("pool", lambda q: q.dma_start(out=QM[h, 0:64, csl], in_=sg[hh * 64:(hh + 1) * 64, :]),
                                 reads=[sgn], writes=[("QM", h, c)])
                  for r in range(2):
                      acc, accn = next_acc()
                      acc2, accn2 = next_acc()
                      for kk in range(2):
                          tk.op("pe", lambda p: p.matmul(acc[:, :], lhsT=wqr_bf[:, kk, r * 128:(r + 1) * 128], rhs=cqn[:, kk, :],
                                                         start=(kk == 0), stop=(kk == 1)), reads=["cqn"] + W_ALL, writes=[accn])
                      for kk in range(2):
                          tk.op("pe", lambda p: p.matmul(acc2[:, :], lhsT=wqt_bf[:, kk, r * 128:(r + 1) * 128], rhs=cqn[:, kk, :],
                                                         start=(kk == 0), stop=(kk == 1)), reads=["cqn"] + W_ALL, writes=[accn2])
                      sg, sgn = next_stg()
                      tk.op("dve", lambda v: v.tensor_tensor(out=t1[:], in0=acc[:, :], in1=cs[:, 1, :], op=ALU.mult), reads=[accn, "cs"], writes=["t1"])
                      tk.op("dve", lambda v: v.tensor_tensor(out=t2[:], in0=acc2[:, :], in1=cs[:, 0, :], op=ALU.mult), reads=[accn2, "cs"], writes=["t2"])
                      tk.op("dve", lambda v: v.tensor_tensor(out=sg[:], in0=t1[:], in1=t2[:], op=ALU.add), reads=["t1", "t2"], writes=[sgn])
                      for jj in range(4):
                          h = 4 * r + jj
                          tk.dma("pool", lambda q: q.dma_start(out=QM[h, 64:96, csl], in_=sg[jj * 32:(jj + 1) * 32, :]),
                                 reads=[sgn], writes=[("QMR", h, c)])
                  chk("A8")
                  for m in range(4):
                      acc, accn = next_acc()
                      tk.op("pe", lambda p: p.matmul(acc[:, :], lhsT=wkn_bf[:, m * 128:(m + 1) * 128], rhs=cqn[:, 2, :], start=True, stop=True),
                            reads=["cqn"] + W_ALL, writes=[accn])
                      sg, sgn = next_stg()
                      evac(sg[:], acc[:, :], [accn], [sgn])
                      for hh in range(2):
                          h = 2 * m + hh
                          tk.dma("pool", lambda q: q.dma_start(out=KM[h, 0:64, csl], in_=sg[hh * 64:(hh + 1) * 64, :]), reads=[sgn], writes=[("KMN", h, c)])
                  for t in range(4):
                      for half in range(2):
                          acc, accn = next_acc()
                          tk.op("pe", lambda p: p.matmul(acc[:, :], lhsT=cqn[:, 2, t * 128:(t + 1) * 128], rhs=wukv_bf[:, half * 512:(half + 1) * 512],
                                                         start=True, stop=True), reads=["cqn"] + W_ALL, writes=[accn])
                          evac(vstm[:, t, half * 4:(half + 1) * 4, 0:64], acc[:, :].rearrange("p (j e) -> p j e", e=128)[:, :, 64:128], [accn], ["vstm"])
                  for h in range(8):
                      tk.dma("pool", lambda q: q.dma_start(out=VM[h, :, 4 * c:4 * c + 4, :], in_=vstm[:, :, h, :]),
                             reads=["vstm"], writes=[("VM", h, c)])

              pro_load(0)
              prologue(0)
              prologue2(0)
              for c in range(NCH):
                  if c + 1 < NCH:
                      pro_load(c + 1)
                  body(c)
                  if c + 1 < NCH:
                      prologue2(c + 1)
              tk.barrier()

          if stop == "A":
              break
          with ExitStack() as pb:
              kt = [sb(pb, "kt%d" % i, [128, S], BF16) for i in range(2)]
              qt = [sb(pb, "qt%d" % i, [128, S], BF16) for i in range(2)]
              vt = [sb(pb, "vt%d" % i, [128, NT, 128], BF16) for i in range(2)]
              pT = [sb(pb, "pT%d" % i, [128, 512], BF16) for i in range(4)]
              num = [sb(pb, "num%d" % i, [65, 512], F32) for i in range(2)]
              sqns = [sb(pb, "sqn%d" % i, [65, 512], BF16) for i in range(2)]
              lnv = sb(pb, "lnv", [64, 512], F32)
              atb = [sb(pb, "atb%d" % i, [64, 512], BF16) for i in range(2)]
              pS = [ps(pb, "pS%d" % i, [128, 512], F32) for i in range(4)]
              pO = [ps(pb, "pO%d" % i, [128, 512], F32) for i in range(2)]
              pSS = ps(pb, "pSS", [128, 512], F32)
              for i in range(2):
                  tk.op("pool", lambda v: v.memset(kt[i][64:96, :], 1.0), writes=[("kt", i)])
                  tk.op("pool", lambda v: v.memset(vt[i][:], 0.0), writes=[("vt", i)])
              LA = 3
              items = [(hh, c, j) for hh in range(16) for c in range(NCH) for j in range(4 * c + 4)]

              def load_head(hh):
                  fox = hh < 8
                  h = hh % 8
                  b2 = hh % 2
                  ktb, qtb, vtb = kt[b2], qt[b2], vt[b2]
                  if fox:
                      tk.dma("sp", lambda q: q.dma_start(out=ktb[0:64, :], in_=KF[h, :, :]),
                             reads=[("QKF", 1, h, c) for c in range(NCH)], writes=[("kt", b2)])
                      tk.dma("sp", lambda q: q.dma_start(out=qtb[0:67, :], in_=QF[h, :, :]),
                             reads=[("QKF", 0, h, c) for c in range(NCH)] + [("QFD", c) for c in range(NCH)], writes=[("qt", b2)])
                      tk.dma("sp", lambda q: q.dma_start(out=vtb[:, :, 0:65], in_=VF[h]), reads=[("VF", h, c) for c in range(NCH)], writes=[("vt", b2)])
                  else:
                      tk.dma("sp", lambda q: q.dma_start(out=ktb[0:96, :], in_=KM[h, :, :]),
                             reads=[("KMR", h, c) for c in range(NCH)] + [("KMN", h, c) for c in range(NCH)], writes=[("kt", b2)])
                      tk.dma("sp", lambda q: q.dma_start(out=qtb[0:96, :], in_=QM[h, :, :]), reads=[("QM", h, c) for c in range(NCH)] + [("QMR", h, c) for c in range(NCH)], writes=[("qt", b2)])
                      tk.dma("sp", lambda q: q.dma_start(out=vtb[:, :, 0:65], in_=VM[h]), reads=[("VM", h, c) for c in range(NCH)], writes=[("vt", b2)])

              def emit_qk(n):
                  hh, c, j = items[n]
                  fox = hh < 8
                  R = 67 if fox else 96
                  b2 = hh % 2
                  ktb, qtb = kt[b2], qt[b2]
                  rd = [("kt", b2), ("qt", b2)]
                  q0 = c * 512
                  psb, psn = pS[n % 4], ("pS", n % 4)
                  ptb, ptn = pT[n % 4], ("pT", n % 4)
                  ksl = slice(j * 128, (j + 1) * 128)
                  if j < 4 * c:
                      tk.op("pe", lambda p: p.matmul(psb[:, :], lhsT=ktb[0:R, ksl], rhs=qtb[0:R, q0:q0 + 512], start=True, stop=True),
                            reads=rd, writes=[psn])
                  else:
                      i = j - 4 * c
                      lo = i * 128
                      tk.op("pe", lambda p: p.matmul(psb[:, lo:lo + 128], lhsT=identb[:], rhs=maskb[:], start=True, stop=False),
                            reads=["identb", "maskb"], writes=[psn])
                      tk.op("pe", lambda p: p.matmul(psb[:, lo:lo + 128], lhsT=ktb[0:R, ksl], rhs=qtb[0:R, q0 + lo:q0 + lo + 128],
                                                     start=False, stop=True), reads=rd, writes=[psn])
                      if i < 3:
                          tk.op("pe", lambda p: p.matmul(psb[:, lo + 128:512], lhsT=ktb[0:R, ksl], rhs=qtb[0:R, q0 + lo + 128:q0 + 512],
                                                         start=True, stop=True), reads=rd, writes=[psn])
                      if lo > 0:
                          tk.op("pool", lambda v: v.memset(ptb[:, 0:lo], 0.0), writes=[ptn])

              def emit_exp_pv(n):
                  hh, c, j = items[n]
                  fox = hh < 8
                  h = hh % 8
                  b2 = hh % 2
                  vtb = vt[b2]
                  q0 = c * 512
                  nk = 4 * c + 4
                  psb, psn = pS[n % 4], ("pS", n % 4)
                  ptb, ptn = pT[n % 4], ("pT", n % 4)
                  po, pon = pO[c % 2], ("pO", c % 2)
                  lo = 0 if j < 4 * c else (j - 4 * c) * 128
                  bias_ap = NDp_all[l][:, j, h:h + 1] if fox else 0.0
                  tk.op("act", lambda a: a.activation(out=ptb[:, lo:512], in_=psb[:, lo:512], func=AF.Exp, bias=bias_ap),
                        reads=[psn, "NDp"], writes=[ptn])
                  tk.op("pe", lambda p: p.matmul(po[:, :], lhsT=vtb[:, j, :], rhs=ptb[:, :], start=(j == 0), stop=(j == nk - 1)),
                        reads=[ptn, ("vt", b2)], writes=[pon])
                  if j == nk - 1:
                      kp = kpost[0] % 2
                      kpost[0] += 1
                      nb, nbn = num[kp], ("num", kp)
                      ab, abn = atb[kp], ("atb", kp)
                      sqn, sqnn = sqns[kp], ("sqn", kp)
                      tk.op("dve", lambda v: v.tensor_copy(out=nb[:], in_=po[0:65, :]), reads=[pon], writes=[nbn])
                      tk.op("pool", lambda v: v.tensor_tensor(out=sqn[:], in0=nb[:], in1=nb[:], op=ALU.mult), reads=[nbn], writes=[sqnn])

                      def p2():
                          tk.op("pe", lambda p: p.matmul(pSS[0:64, :], lhsT=wselb[:], rhs=sqn[:], start=True, stop=True), reads=[sqnn, "wselb"], writes=["pSS"])

                      def p3():
                          tk.op("act", lambda a: a.activation(out=lnv[:], in_=pSS[0:64, :], func=AF.Ln), reads=["pSS"], writes=["lnv"])
                          tk.op("act", lambda a: a.activation(out=lnv[:], in_=lnv[:], func=AF.Exp, scale=-0.5), reads=["lnv"], writes=["lnv"])

                      def p4():
                          tk.op("dve", lambda v: v.tensor_tensor(out=ab[:], in0=nb[0:64, :], in1=lnv[:], op=ALU.mult), reads=[nbn, "lnv"], writes=[abn])
                          tk.dma("sp", lambda q: q.dma_start(out=AT[hh, :, q0:q0 + 512], in_=ab[:]), reads=[abn], writes=[("AT", hh, c)])

                      deferred.append((step[0] + 7, p2))
                      deferred.append((step[0] + 10, p3))
                      deferred.append((step[0] + 12, p4))

              load_head(0)
              NI = len(items)
              deferred = []
              step = [0]
              kpost = [0]
              for n in range(NI + LA + 16):
                  step[0] = n
                  due = [d for d in deferred if d[0] <= n]
                  for d in due:
                      deferred.remove(d)
                      d[1]()
                  if n < NI:
                      hh, c, j = items[n]
                      if c == 0 and j == 0 and hh + 1 < 16:
                          pass
                      if c == 1 and j == 0 and hh + 1 < 16:
                          load_head(hh + 1)
                      emit_qk(n)
                  if LA <= n < NI + LA:
                      emit_exp_pv(n - LA)
              assert not deferred
              tk.barrier()

          if stop == "B":
              break
          with ExitStack() as pc:
              gO = sb(pc, "gO", [128, 8], F32)
              gF = sb(pc, "gF", [128, 8], F32)
              wrt = sb(pc, "wrt", [128, 8, 40], F32)
              XNT = sb(pc, "XNT", [128, 8, S], BF16)
              M1 = sb(pc, "M1", [128, NT, 32], F32)
              M2 = sb(pc, "M2", [128, NT, 32], F32)
              G1 = sb(pc, "G1", [128, NT], F32)
              G2 = sb(pc, "G2", [128, NT], F32)
              tk.dma("sp", lambda q: q.dma_start(out=gO[:], in_=gO_in[l]), writes=["gO"])
              tk.dma("sp", lambda q: q.dma_start(out=gF[:], in_=gF_in[l]), writes=["gF"])
              with ExitStack() as pc1:
                  wo_bf = sb(pc1, "wo_bf", [128, 8, D], BF16)
                  wst = sb(pc1, "wst", [128, D], F32)
                  atts = [sb(pc1, "att%d" % i, [128, 8, 512], BF16) for i in range(2)]
                  hxcs = [sb(pc1, "hxC%d" % i, [128, 4, D], F32) for i in range(2)]
                  h1 = [sb(pc1, "h1_%d" % i, [128, D], F32) for i in range(2)]
                  junk = sb(pc1, "junkC", [128, D], F32)
                  xn2s = [sb(pc1, "xn2_%d" % i, [128, D], F32) for i in range(2)]
                  smxs = [sb(pc1, "smx%d" % i, [128, 2], F32) for i in range(2)]
                  xT32s = [sb(pc1, "xT32_%d" % i, [128, 8, 128], F32) for i in range(2)]
                  sm = sb(pc1, "sm", [128, 16], F32)
                  lg = sb(pc1, "lg", [128, 40], F32)
                  r8 = [sb(pc1, "r8_%d" % i, [128, 8], F32) for i in range(3)]
                  r32 = sb(pc1, "r32", [128, 8, 4], F32)
                  r4 = [sb(pc1, "r4_%d" % i, [128, 4], F32) for i in range(5)]
                  py = [ps(pc1, "py%d" % i, [128, 512], F32) for i in range(4)]
                  ptx = [ps(pc1, "ptx%d" % i, [128, 512], F32) for i in range(2)]
                  plg = ps(pc1, "plg", [128, 512], F32)
                  def c1_load(c):
                      q0 = c * 512
                      att, hx = atts[c % 2], hxcs[c % 2]
                      tk.dma("sp", lambda q: q.dma_start(out=att[:], in_=AT[:, :, q0:q0 + 512].rearrange("(m a) p n -> (a p) m n", a=2)),
                             reads=[("AT", hh, c) for hh in range(16)], writes=[("att", c % 2)])
                      tk.dma("sp", lambda q: q.dma_start(out=hx[:], in_=hsrc[q0:q0 + 512, :].rearrange("(t p) d -> p t d", p=128)),
                             reads=[("hB", l, 4 * c + t_) for t_ in range(4)], writes=[("hxC", c % 2)])

                  c1_load(0)
                  for hh in range(8):
                      tk.dma("sp", lambda q: q.dma_start(out=wst[:], in_=wo_in[l, :, hh, :]), writes=["wst"])
                      tk.op("dve", lambda v: v.tensor_scalar(out=wo_bf[:, hh, :], in0=wst[:], scalar1=gO[:, hh:hh + 1], scalar2=None, op0=ALU.mult),
                            reads=["wst", "gO"], writes=["wo_bf"])
                  tk.dma("sp", lambda q: q.dma_start(out=wrt[:], in_=wrt_in[l]), writes=["wrt"])
                  for k in range(8):
                      tk.op("dve", lambda v: v.tensor_scalar(out=wrt[:, k, :], in0=wrt[:, k, :], scalar1=gF[:, k:k + 1], scalar2=None, op0=ALU.mult),
                            reads=["wrt", "gF"], writes=["wrt"])
                  def stageX(T):
                      c, t = T // 4, T % 4
                      q0 = c * 512
                      xn2 = xn2s[T % 2]
                      XN2 = ("xn2", T % 2)
                      smx = smxs[T % 2]
                      SMX = ("smx", T % 2)
                      att, hx = atts[c % 2], hxcs[c % 2]
                      ATT, HXC = ("att", c % 2), ("hxC", c % 2)
                      if t == 0 and c + 1 < NCH:
                          c1_load(c + 1)
                      hb, hbn = h1[T % 2], ("h1", T % 2)
                      for half in range(2):
                          pyb, pyn = py[(T % 2) * 2 + half], ("py", (T % 2) * 2 + half)
                          for hh in range(8):
                              tk.op("pe", lambda p: p.matmul(pyb[:, :], lhsT=att[:, hh, t * 128:(t + 1) * 128],
                                                             rhs=wo_bf[:, hh, half * 512:(half + 1) * 512], start=(hh == 0), stop=(hh == 7)),
                                    reads=[ATT, "wo_bf"], writes=[pyn])
                          tk.op("dve", lambda v: v.tensor_tensor(out=hb[:, half * 512:(half + 1) * 512], in0=pyb[:, :],
                                                                 in1=hx[:, t, half * 512:(half + 1) * 512], op=ALU.add),
                                reads=[pyn, HXC], writes=[hbn])
                      tk.dma("sp", lambda q: q.dma_start(out=hcur[T * 128:(T + 1) * 128, :], in_=hb[:]), reads=[hbn], writes=[("hA", T)])
                      tk.op("act", lambda a: a.activation(out=junk[:], in_=hb[:], func=AF.Square), reads=[hbn], writes=["junkC"])
                      tk.op("dve", lambda v: v.tensor_reduce(out=smx[:, 0:1], in_=junk[:], axis=AX.X, op=ALU.add), reads=["junkC"], writes=[SMX])
                      tk.op("act", lambda a: a.activation(out=smx[:, 1:2], in_=smx[:, 0:1], func=AF.Ln, bias=EPS, scale=1.0 / D), reads=[SMX], writes=[SMX])
                      tk.op("act", lambda a: a.activation(out=smx[:, 1:2], in_=smx[:, 1:2], func=AF.Exp, scale=-0.5), reads=[SMX], writes=[SMX])
                      tk.op("dve", lambda v: v.tensor_scalar(out=xn2[:], in0=hb[:], scalar1=smx[:, 1:2], scalar2=None, op0=ALU.mult),
                            reads=[hbn, SMX], writes=[XN2])

                  def stageY(T):
                      c, t = T // 4, T % 4
                      xn2 = xn2s[T % 2]
                      XN2 = ("xn2", T % 2)
                      xT32 = xT32s[T % 2]
                      XT32 = ("xT32", T % 2)
                      for k in range(8):
                          pb_, pbn = ptx[k // 4], ("ptx", k // 4)
                          tk.op("pe", lambda p: p.transpose(out=pb_[:, (k % 4) * 128:(k % 4 + 1) * 128], in_=xn2[:, k * 128:(k + 1) * 128],
                                                           identity=identf), reads=[XN2, "cst"], writes=[pbn])
                          if k % 4 == 3:
                              kk0 = k - 3
                              tk.op("act", lambda a: a.copy(out=xT32[:, kk0:kk0 + 4, :], in_=pb_[:, :].rearrange("p (a n) -> p a n", n=128)),
                                    reads=[pbn], writes=[XT32])
                      tk.op("pool", lambda v: v.tensor_copy(out=XNT[:, :, T * 128:(T + 1) * 128], in_=xT32[:]), reads=[XT32], writes=[("XNT", T)])

                  def stageY2(T):
                      c, t = T // 4, T % 4
                      xT32 = xT32s[T % 2]
                      XT32 = ("xT32", T % 2)
                      for k in range(8):
                          tk.op("pe", lambda p: p.matmul(plg[:, 0:40], lhsT=xT32[:, k, :], rhs=wrt[:, k, :], start=(k == 0), stop=(k == 7)),
                                reads=[XT32, "wrt"], writes=["plg"])
                      V = "dve"
                      tk.op(V, lambda v: v.tensor_copy(out=lg[:], in_=plg[:, 0:40]), reads=["plg"], writes=["rt"])
                      R_ = dict(reads=["rt"], writes=["rt"])
                      tk.op(V, lambda v: v.tensor_reduce(out=sm[:, 2:3], in_=lg[:, 0:8], axis=AX.X, op=ALU.max), **R_)
                      tk.op(V, lambda v: v.tensor_scalar(out=r8[0][:], in0=lg[:, 0:8], scalar1=sm[:, 2:3], scalar2=None, op0=ALU.is_equal), **R_)
                      tk.op(V, lambda v: v.tensor_scalar(out=sm[:, 3:4], in0=sm[:, 2:3], scalar1=-1.0, scalar2=None, op0=ALU.mult), **R_)
                      tk.op("act", lambda a: a.activation(out=r8[1][:], in_=lg[:, 0:8], func=AF.Exp, bias=sm[:, 3:4]), **R_)
                      tk.op(V, lambda v: v.tensor_reduce(out=sm[:, 4:5], in_=r8[1][:], axis=AX.X, op=ALU.add), **R_)
                      tk.op(V, lambda v: v.reciprocal(out=sm[:, 5:6], in_=sm[:, 4:5]), **R_)
                      elv = lg[:, 8:40].rearrange("p (g j) -> p g j", j=4)
                      tk.op(V, lambda v: v.tensor_tensor(out=r32[:], in0=elv, in1=r8[0][:].unsqueeze(2).to_broadcast([128, 8, 4]), op=ALU.mult), **R_)
                      tk.op(V, lambda v: v.tensor_reduce(out=r4[0][:], in_=r32[:].rearrange("p g j -> p j g"), axis=AX.X, op=ALU.add), **R_)
                      tk.op(V, lambda v: v.tensor_reduce(out=sm[:, 6:7], in_=r4[0][:], axis=AX.X, op=ALU.max), **R_)
                      tk.op(V, lambda v: v.tensor_scalar(out=r4[1][:], in0=r4[0][:], scalar1=sm[:, 6:7], scalar2=None, op0=ALU.is_equal), **R_)
                      tk.op(V, lambda v: v.scalar_tensor_tensor(out=r4[2][:], in0=r4[1][:], scalar=-1e30, in1=r4[0][:], op0=ALU.mult, op1=ALU.add), **R_)
                      tk.op(V, lambda v: v.tensor_reduce(out=sm[:, 7:8], in_=r4[2][:], axis=AX.X, op=ALU.max), **R_)
                      tk.op(V, lambda v: v.tensor_scalar(out=r4[3][:], in0=r4[2][:], scalar1=sm[:, 7:8], scalar2=None, op0=ALU.is_equal), **R_)
                      tk.op(V, lambda v: v.tensor_tensor(out=sm[:, 8:9], in0=sm[:, 7:8], in1=sm[:, 6:7], op=ALU.subtract), **R_)
                      tk.op("act", lambda a: a.activation(out=sm[:, 9:10], in_=sm[:, 8:9], func=AF.Exp), **R_)
                      tk.op(V, lambda v: v.tensor_scalar(out=sm[:, 10:11], in0=sm[:, 9:10], scalar1=1.0, scalar2=None, op0=ALU.add), **R_)
                      tk.op(V, lambda v: v.reciprocal(out=sm[:, 11:12], in_=sm[:, 10:11]), **R_)
                      tk.op(V, lambda v: v.tensor_tensor(out=G1[:, T:T + 1], in0=sm[:, 11:12], in1=sm[:, 5:6], op=ALU.mult), reads=["rt"], writes=["rt", "G"])
                      tk.op(V, lambda v: v.scalar_tensor_tensor(out=G2[:, T:T + 1], in0=sm[:, 9:10], scalar=sm[:, 11:12], in1=sm[:, 5:6],
                                                                op0=ALU.mult, op1=ALU.mult), reads=["rt"], writes=["rt", "G"])
                      m1v = M1[:, T, :].rearrange("p (g j) -> p g j", j=4)
                      m2v = M2[:, T, :].rearrange("p (g j) -> p g j", j=4)
                      ohb = r8[0][:].unsqueeze(2).to_broadcast([128, 8, 4])
                      tk.op(V, lambda v: v.tensor_tensor(out=m1v, in0=ohb, in1=r4[1][:].unsqueeze(1).to_broadcast([128, 8, 4]), op=ALU.mult),
                            reads=["rt"], writes=["rt", "M"])
                      tk.op(V, lambda v: v.tensor_tensor(out=m2v, in0=ohb, in1=r4[3][:].unsqueeze(1).to_broadcast([128, 8, 4]), op=ALU.mult),
                            reads=["rt"], writes=["rt", "M"])

                  stageX(0)
                  for T in range(NT + 1):
                      if T + 1 < NT:
                          stageX(T + 1)
                      if T < NT:
                          stageY(T)
                      if T >= 1:
                          stageY2(T - 1)
                  tk.barrier()

              if stop == "C":
                  break
              with ExitStack() as pd:
                  GT = sb(pd, "GT", [128, NT, 32], F32)
                  wguf = [sb(pd, "wguf%d" % i, [128, 8, 512], F32) for i in range(2)]
                  wdf = [sb(pd, "wdf%d" % i, [128, 2, D], F32) for i in range(2)]
                  wgub = [sb(pd, "wgub%d" % i, [128, 8, 512], BF16) for i in range(2)]
                  wdb = [sb(pd, "wdb%d" % i, [128, 2, D], BF16) for i in range(2)]
                  sg_ = [sb(pd, "sgD%d" % i, [128, 512], F32) for i in range(2)]
                  hT = [sb(pd, "hT%d" % i, [128, 2, 512], BF16) for i in range(2)]
                  yacc = sb(pd, "yacc", [128, 8, D], F32)
                  smf = sb(pd, "smf", [128, 4], F32)
                  if last:
                      hh1 = [sb(pd, "hh1_%d" % i, [128, D], F32) for i in range(1)]
                      junk = sb(pd, "junkF", [128, D], F32)
                      fing = sb(pd, "fing", [128, D], F32)
                  else:
                      tmpy = [sb(pd, "tmpy%d" % i, [128, 512], F32) for i in range(2)]
                  pgu = [ps(pd, "pgu%d" % i, [128, 512], F32) for i in range(4)]
                  pyd = [ps(pd, "pyd%d" % i, [128, 512], F32) for i in range(4)]
                  if last:
                      tk.dma("sp", lambda q: q.dma_start(out=fing[:], in_=fin_in[:, :]), writes=["fing"])
                  for T in range(NT):
                      tk.op("dve", lambda v: v.tensor_scalar(out=GT[:, T, :], in0=M1[:, T, :], scalar1=G1[:, T:T + 1], scalar2=None, op0=ALU.mult),
                            reads=["M", "G"], writes=["GT"])
                      tk.op("dve", lambda v: v.scalar_tensor_tensor(out=GT[:, T, :], in0=M2[:, T, :], scalar=G2[:, T:T + 1], in1=GT[:, T, :],
                                                                    op0=ALU.mult, op1=ALU.add), reads=["M", "G", "GT"], writes=["GT"])

                  def load_w(n):
                      e = n % 32
                      i = n % 2
                      tk.dma("sp", lambda q: q.dma_start(out=wguf[i][:].rearrange("p k n -> p (k n)"), in_=wgu_in[l][e * 128:(e + 1) * 128, :]),
                             writes=[("wguf", i)])
                      tk.dma("sp", lambda q: q.dma_start(out=wdf[i][:].rearrange("p k n -> p (k n)"), in_=wd_in[l][e * 128:(e + 1) * 128, :]),
                             writes=[("wdf", i)])

                  GS = 8
                  NG = NT // GS
                  load_w(0)
                  wcnt = [0]

                  def stage1(grp, e, cc, n, i, fs=range(4)):
                      t0_ = (grp * GS + cc * 4) * 128
                      hb = hT[n % 2]
                      for f in fs:
                          pg, pgn = pgu[f], ("pgu", f)
                          for k in range(8):
                              tk.op("pe", lambda p: p.matmul(pg[:, :], lhsT=wgub[i][:, k, f * 128:(f + 1) * 128], rhs=XNT[:, k, t0_:t0_ + 512],
                                                             start=(k == 0), stop=(k == 7)), reads=[("XNT", grp * GS + cc * 4 + t_) for t_ in range(4)] + [("wgub", i)], writes=[pgn])
                          if f < 2:
                              tk.op("act", lambda a: a.activation(out=sg_[f][:], in_=pg[:, :], func=AF.Silu), reads=[pgn], writes=[("sgD", f)])
                          else:
                              tk.op("dve", lambda v: v.tensor_tensor(out=hb[:, f - 2, :], in0=sg_[f - 2][:], in1=pg[:, :], op=ALU.mult),
                                    reads=[("sgD", f - 2), pgn], writes=[("hT", n % 2)])

                  def stage3(grp, e, cc, n, i, ts=range(4)):
                      hb = hT[n % 2]
                      for t in ts:
                          tt = cc * 4 + t
                          T = grp * GS + tt
                          for half in range(2):
                              q_ = (t * 2 + half) % 4
                              pq_, pqn = pyd[q_], ("pyd", q_)
                              for kk in range(2):
                                  tk.op("pe", lambda p: p.matmul(pq_[:, :], lhsT=hb[:, kk, t * 128:(t + 1) * 128], rhs=wdb[i][:, kk, half * 512:(half + 1) * 512],
                                                                 start=(kk == 0), stop=(kk == 1)), reads=[("hT", n % 2), ("wdb", i)], writes=[pqn])
                              ysl = yacc[:, tt, half * 512:(half + 1) * 512]
                              YR = ("yacc", tt, half)
                              if False and (not last) and half == 1:
                                  if e == 0:
                                      tk.op("act", lambda a: a.activation(out=ysl, in_=pq_[:, :], func=AF.Copy, scale=GT[:, T, e:e + 1]),
                                            reads=[pqn, "GT"], writes=[YR])
                                  else:
                                      tb, tbn = tmpy[t % 2], ("tmpy", t % 2)
                                      tk.op("act", lambda a: a.activation(out=tb[:], in_=pq_[:, :], func=AF.Copy, scale=GT[:, T, e:e + 1]),
                                            reads=[pqn, "GT"], writes=[tbn])
                                      tk.op("pool", lambda v: v.tensor_tensor(out=ysl, in0=ysl, in1=tb[:], op=ALU.add), reads=[tbn, YR], writes=[YR])
                              elif e == 0:
                                  tk.op("dve", lambda v: v.tensor_scalar(out=ysl, in0=pq_[:, :], scalar1=GT[:, T, e:e + 1], scalar2=None, op0=ALU.mult),
                                        reads=[pqn, "GT"], writes=[YR])
                              else:
                                  tk.op("dve", lambda v: v.scalar_tensor_tensor(out=ysl, in0=pq_[:, :], scalar=GT[:, T, e:e + 1], in1=ysl,
                                                                                op0=ALU.mult, op1=ALU.add),
                                        reads=[pqn, YR, "GT"], writes=[YR])

                  def final_norm_group(g_):
                      for tt in range(GS):
                          T = g_ * GS + tt
                          i2 = 0
                          tk.dma("sp", lambda q: q.dma_start(out=hh1[i2][:], in_=hcur[T * 128:(T + 1) * 128, :]), reads=[("hB", l + 1, T)], writes=[("hh1", i2)])
                          tk.op("act", lambda a: a.activation(out=junk[:], in_=hh1[i2][:], func=AF.Square), reads=[("hh1", i2)], writes=["junkF"])
                          tk.op("dve", lambda v: v.tensor_reduce(out=smf[:, 0:1], in_=junk[:], axis=AX.X, op=ALU.add), reads=["junkF"], writes=["smf"])
                          tk.op("act", lambda a: a.activation(out=smf[:, 1:2], in_=smf[:, 0:1], func=AF.Ln, bias=EPS, scale=1.0 / D), reads=["smf"], writes=["smf"])
                          tk.op("act", lambda a: a.activation(out=smf[:, 1:2], in_=smf[:, 1:2], func=AF.Exp, scale=-0.5), reads=["smf"], writes=["smf"])
                          tk.op("dve", lambda v: v.scalar_tensor_tensor(out=hh1[i2][:], in0=hh1[i2][:], scalar=smf[:, 1:2], in1=fing[:],
                                                                        op0=ALU.mult, op1=ALU.mult), reads=[("hh1", i2), "smf", "fing"], writes=[("hh1", i2)])
                          tk.dma("sp", lambda q: q.dma_start(out=y_out[T * 128:(T + 1) * 128, :], in_=hh1[i2][:]), reads=[("hh1", i2)], writes=[("out", T)])

                  NQ = NG * 32

                  def cast_w(q):
                      i = q % 2
                      for k in range(8):
                          tk.op("act", lambda a: a.activation(out=wgub[i][:, k, :], in_=wguf[i][:, k, :], func=AF.Copy, scale=gF[:, k:k + 1]),
                                reads=[("wguf", i), "gF"], writes=[("wgub", i)])
                      tk.op("pool", lambda v: v.tensor_copy(out=wdb[i][:], in_=wdf[i][:]), reads=[("wdf", i)], writes=[("wdb", i)])

                  load_w(1)
                  cast_w(0)
                  for grp in range(NG):
                      its = [(e, cc) for e in range(32) for cc in range(GS // 4)]
                      NI2 = len(its)
                      for n in range(NI2 + 1):
                          for f in range(4):
                              if n < NI2:
                                  e, cc = its[n]
                                  stage1(grp, e, cc, n, (grp * 32 + e) % 2, fs=[f])
                              if n >= 1:
                                  e2, cc2 = its[n - 1]
                                  stage3(grp, e2, cc2, n - 1, (grp * 32 + e2) % 2, ts=[f])
                          if n < NI2 and its[n][1] == 0:
                              q = grp * 32 + its[n][0]
                              if q + 1 < NQ:
                                  cast_w(q + 1)
                              if q + 2 < NQ:
                                  load_w(q + 2)
                      for tt in range(GS):
                          T = grp * GS + tt
                          YRS = [("yacc", tt, 0), ("yacc", tt, 1)]
                          tk.dma("pool", lambda q: q.dma_start(out=hcur[T * 128:(T + 1) * 128, :], in_=yacc[:, tt, :], accum_op=ALU.add),
                                 reads=YRS + [("hA", T)], writes=[("hB", l + 1, T)])
                      if last:
                          if grp >= 1:
                              final_norm_group(grp - 1)
                  if last:
                      final_norm_group(NG - 1)
                  tk.barrier()
          if stop == "L0":
              break
    except _Stop:
        return nc
    es.close()
    return nc


def _host_layout(inp):
    f = np.float32
    g = {k: np.asarray(v) for k, v in inp.items()}
    sh = {}
    sh["gA"] = np.ascontiguousarray(g["attn_norm"].reshape(L, 8, 128).transpose(0, 2, 1)).astype(f)
    sh["gF"] = np.ascontiguousarray(g["ffn_norm"].reshape(L, 8, 128).transpose(0, 2, 1)).astype(f)
    sh["gQ"] = np.ascontiguousarray(g["q_norm"].reshape(L, 2, 128).transpose(0, 2, 1)).astype(f)
    sh["gKV"] = np.ascontiguousarray(g["kv_norm"].reshape(L, 128, 1)).astype(f)
    go = np.concatenate([g["fox_out_norm"], g["mla_out_norm"]], axis=1)
    sh["gO"] = np.ascontiguousarray(go.reshape(L, 8, 128).transpose(0, 2, 1)).astype(f)
    sh["bfb"] = np.ascontiguousarray(np.tile(g["b_f"][:, None, None, :], (1, 128, 4, 1)).reshape(L, 128, 32)).astype(f)
    sh["fing"] = np.ascontiguousarray(np.tile(g["final_norm"][None, :], (128, 1))).astype(f)
    w_in = g["w_in"]
    kr = w_in[:, :, 1928:1960]
    kr_sw = np.concatenate([kr[:, :, 16:32], kr[:, :, 0:16]], axis=2)
    win = np.concatenate([w_in, kr_sw], axis=2)
    sh["win"] = np.ascontiguousarray(win.reshape(L, 8, 128, INW + 32).transpose(0, 2, 1, 3)).astype(f)
    wuq = g["w_uq"]
    rope = wuq.reshape(L, 256, 8, 96)[:, :, :, 64:96]
    rope_sw = np.concatenate([rope[..., 16:32], rope[..., 0:16]], axis=-1).reshape(L, 256, 256)
    wuq2 = np.concatenate([wuq, rope_sw], axis=2)
    sh["wuq"] = np.ascontiguousarray(wuq2.reshape(L, 2, 128, 1024).transpose(0, 2, 1, 3)).astype(f)
    sh["wukv"] = np.ascontiguousarray(g["w_ukv"]).astype(f)
    sh["wo"] = np.ascontiguousarray(g["w_o"].reshape(L, 8, 128, 1024).transpose(0, 2, 1, 3)).astype(f)
    wrt = np.concatenate([g["w_group"], g["w_router"]], axis=2)
    sh["wrt"] = np.ascontiguousarray(wrt.reshape(L, 8, 128, 40).transpose(0, 2, 1, 3)).astype(f)
    for l in range(L):
        gu = np.concatenate([g["w_gate"][l], g["w_up"][l]], axis=2)
        sh["wgu%d" % l] = np.ascontiguousarray(gu.reshape(32, 8, 128, 512).transpose(0, 2, 1, 3).reshape(4096, 4096)).astype(f)
        wd = g["w_down"][l]
        sh["wd%d" % l] = np.ascontiguousarray(wd.reshape(32, 2, 128, 1024).transpose(0, 2, 1, 3).reshape(4096, 2048)).astype(f)
    cst = np.zeros((128, 6, 128), f)
    cst[:, 0, :] = np.eye(128, dtype=f)
    ii = np.arange(128)
    cst[:, 1, :] = (ii[:, None] <= ii[None, :]).astype(f)
    cst[:, 2, :] = (ii[:, None] < ii[None, :]).astype(f)
    cst[:, 3, :] = 1.0
    cst[:, 4, :] = np.where(ii[:, None] > ii[None, :], -30000.0, 0.0).astype(f)
    cst[0:64, 5, 0:64] = 1.0 / 64.0
    cst[64, 5, 0:64] = EPS
    sh["cst"] = cst
    cst2 = np.zeros((128, 4 + NBLK), f)
    half = 16
    inv_freq = (10000.0 ** (-np.arange(half, dtype=np.float64) / half))
    cst2[:, 0] = (inv_freq[ii % 16] / (2.0 * np.pi)).astype(f)
    cst2[:, 1] = ii.astype(f)
    cst2[:, 4:] = (np.arange(NBLK) * 128).astype(f)[None, :]
    sh["cst2"] = cst2
    return sh


_CACHE = {}


def kernel(**inputs):
    x = np.asarray(inputs["x"], dtype=np.float32)
    pos = np.asarray(inputs["positions"]).astype(np.int32)
    shared = _host_layout(inputs)
    if "nc" not in _CACHE:
        _CACHE["nc"] = build_program()
    nc = _CACHE["nc"]
    in_maps = []
    for b in range(8):
        m = dict(shared)
        m["x"] = np.ascontiguousarray(x[b])
        m["pos"] = np.ascontiguousarray(np.tile(pos[b][None, :], (128, 1)))
        in_maps.append(m)
    res = run_bass_kernel_spmd(nc, in_maps, core_ids=list(range(8)))
    out = np.stack([np.asarray(r["y"]) for r in res.results], axis=0)
    return out.astype(np.float32)
```
